# Optimizing a Trainium2 kernel written in Bass

```python
import math
import jax, jax.numpy as jnp
from jax import lax
import numpy as np

D_MODEL = 2048
BATCH = 4
SEQ = 2048
DEPTH = 2
DEC_BATCH = 128
DEC_SEQ = 8
PAST_LEN = 16384
PAGE_SIZE = 128

CONV_WIDTH = D_MODEL // 4
POOL_WIDTH = D_MODEL // 4
RET_WIDTH = D_MODEL // 2
CONV_K = 3
POOL_WINDOWS = (2, 4, 8, 16)
N_POOL_GROUPS = len(POOL_WINDOWS)
POOL_GROUP = POOL_WIDTH // N_POOL_GROUPS
POOL_HIST = max(POOL_WINDOWS) - 1
RET_HEADS = 4
RET_HEAD_DIM = RET_WIDTH // RET_HEADS
RET_CHUNK = 128
RET_LOG_GAMMA = tuple(math.log(1.0 - 2.0 ** (-5 - h)) for h in range(RET_HEADS))
ROPE_BASE = 10000.0
D_FF = 11 * D_MODEL // 4
PLE_DIM = 256
MIX_WIDTH = CONV_WIDTH + POOL_WIDTH + RET_WIDTH
IN_COLS = 3 * CONV_WIDTH + POOL_WIDTH + 4 * RET_WIDTH
IN_SPLITS = (CONV_WIDTH, 2 * CONV_WIDTH, 3 * CONV_WIDTH,
             3 * CONV_WIDTH + POOL_WIDTH,
             3 * CONV_WIDTH + POOL_WIDTH + RET_WIDTH,
             3 * CONV_WIDTH + POOL_WIDTH + 2 * RET_WIDTH,
             3 * CONV_WIDTH + POOL_WIDTH + 3 * RET_WIDTH)
DEEPNORM_ALPHA = (2 * DEPTH) ** 0.25
DEEPNORM_BETA = (8 * DEPTH) ** -0.25
LN_EPS = 1e-5

kernel_name = "hybrid_conv_pool_retention_decoder_step"


def layer_norm(x, g, b):
    xf = x.astype(jnp.float32)
    mu = jnp.mean(xf, axis=-1, keepdims=True)
    var = jnp.mean(jnp.square(xf - mu), axis=-1, keepdims=True)
    y = (xf - mu) * lax.rsqrt(var + LN_EPS) * g.astype(jnp.float32) + b.astype(jnp.float32)
    return y.astype(x.dtype)


def swiglu(x, w_gate, w_up, w_down):
    return (jax.nn.silu(x @ w_gate) * (x @ w_up)) @ w_down


def rotary(x, pos):
    half = x.shape[-1] // 2
    inv = ROPE_BASE ** (-jnp.arange(half, dtype=jnp.float32) / half)
    ang = pos[:, None] * inv[None, :]
    cos = jnp.cos(ang)[None, :, None, :]
    sin = jnp.sin(ang)[None, :, None, :]
    x1, x2 = x[..., :half], x[..., half:]
    return jnp.concatenate([x1 * cos - x2 * sin, x1 * sin + x2 * cos], axis=-1)


def short_conv_group(z_b, z_c, z_h, prefix, conv_w):
    u = z_c * z_h
    full = jnp.concatenate([prefix.astype(u.dtype), u], axis=1)
    L = u.shape[1]
    conv = sum(full[:, j:j + L] * conv_w[j] for j in range(CONV_K))
    return z_b * conv, full[:, -(CONV_K - 1):]


def pool_group(u, prefix, pool_w, pool_scale, start_pos):
    Bn, L, C = u.shape
    full = jnp.concatenate([prefix.astype(u.dtype), u], axis=1)
    ff = full.astype(jnp.float32)
    cs = jnp.concatenate([jnp.zeros((Bn, 1, C), jnp.float32), jnp.cumsum(ff, axis=1)], axis=1)
    pos = start_pos + jnp.arange(L, dtype=jnp.float32)
    o = POOL_HIST + 1
    means = []
    for gi, w in enumerate(POOL_WINDOWS):
        sl = slice(gi * POOL_GROUP, (gi + 1) * POOL_GROUP)
        win_sum = cs[:, o:o + L, sl] - cs[:, o - w:o - w + L, sl]
        cnt = jnp.minimum(jnp.float32(w), pos + 1.0)[None, :, None]
        means.append(win_sum / cnt)
    d = (jnp.concatenate(means, axis=-1) - u.astype(jnp.float32)).reshape(Bn, L, N_POOL_GROUPS, POOL_GROUP)
    y = jnp.einsum('blgc,gcd->blgd', d, pool_w.astype(jnp.float32)).reshape(Bn, L, POOL_WIDTH)
    y = y * pool_scale.astype(jnp.float32)
    return y.astype(u.dtype), full[:, -POOL_HIST:]


def retention_chunk(S, q, k, v, lg):
    L = q.shape[1]
    idx = jnp.arange(L, dtype=jnp.float32)
    diff = idx[:, None] - idx[None, :]
    causal = diff >= 0
    decay = jnp.where(causal[None], jnp.exp(jnp.where(causal, diff, 0.0)[None] * lg[:, None, None]), 0.0)
    scores = jnp.einsum('blhd,bmhd->bhlm', q, k) * decay[None]
    inner = jnp.einsum('bhlm,bmhe->blhe', scores, v)
    q_decay = jnp.exp((idx[:, None] + 1.0) * lg[None, :])
    cross = jnp.einsum('blhd,bhde->blhe', q, S) * q_decay[None, :, :, None]
    k_decay = jnp.exp((L - 1.0 - idx)[:, None] * lg[None, :])
    S_new = (jnp.exp(L * lg)[None, :, None, None] * S
             + jnp.einsum('blhd,blhe->bhde', k * k_decay[None, :, :, None], v))
    return S_new, inner + cross


def retention_group(zq, zk, zv, zg, S0, start_pos):
    Bn, L, _ = zq.shape
    shp = (Bn, L, RET_HEADS, RET_HEAD_DIM)
    pos = start_pos + jnp.arange(L, dtype=jnp.float32)
    q = rotary(zq.astype(jnp.float32).reshape(shp), pos)
    k = rotary(zk.astype(jnp.float32).reshape(shp), pos) * (RET_HEAD_DIM ** -0.5)
    v = zv.astype(jnp.float32).reshape(shp)
    lg = jnp.array(RET_LOG_GAMMA, dtype=jnp.float32)
    S0 = S0.astype(jnp.float32)
    c = min(L, RET_CHUNK)
    n = L // c
    if n == 1:
        S, o = retention_chunk(S0, q, k, v, lg)
    else:
        def to_chunks(t):
            return jnp.moveaxis(t.reshape(Bn, n, c, RET_HEADS, RET_HEAD_DIM), 1, 0)

        def step(S, xs):
            qc, kc, vc = xs
            return retention_chunk(S, qc, kc, vc, lg)

        S, oc = lax.scan(step, S0, (to_chunks(q), to_chunks(k), to_chunks(v)))
        o = jnp.moveaxis(oc, 0, 1).reshape(shp)
    mu = jnp.mean(o, axis=-1, keepdims=True)
    var = jnp.mean(jnp.square(o - mu), axis=-1, keepdims=True)
    on = ((o - mu) * lax.rsqrt(var + LN_EPS)).reshape(Bn, L, RET_WIDTH)
    y = on * jax.nn.silu(zg.astype(jnp.float32))
    return y.astype(zq.dtype), S


def mixing_sublayer(x, w_in, conv_w, pool_w, pool_scale, w_out, conv_prefix, pool_prefix, ret_state, start_pos):
    z = x @ w_in
    z_b, z_c, z_h, z_p, zq, zk, zv, zg = jnp.split(z, list(IN_SPLITS), axis=-1)
    ya, conv_new = short_conv_group(z_b, z_c, z_h, conv_prefix, conv_w)
    yb, pool_new = pool_group(z_p, pool_prefix, pool_w, pool_scale, start_pos)
    yc, ret_new = retention_group(zq, zk, zv, zg, ret_state, start_pos)
    m = jnp.concatenate([ya, yb, yc], axis=-1) @ w_out
    return m, conv_new, pool_new, ret_new


def trunk(x, p, conv_cache, pool_cache, ret_cache, start_pos,
          ln1_g, ln1_b, ffn1_w_gate, ffn1_w_up, ffn1_w_down, w_in, conv_w, pool_w, pool_scale,
          w_out, ln2_g, ln2_b, ffn2_w_gate, ffn2_w_up, ffn2_w_down, ple_gate, ple_proj, ln3_g, ln3_b):
    convs, pools, rets = [], [], []
    for i in range(DEPTH):
        h = 0.5 * swiglu(x, ffn1_w_gate[i], ffn1_w_up[i], ffn1_w_down[i])
        x = layer_norm(DEEPNORM_ALPHA * x + h, ln1_g[i], ln1_b[i])
        m, c_new, p_new, r_new = mixing_sublayer(x, w_in[i], conv_w[i], pool_w[i], pool_scale[i], w_out[i],
                                                 conv_cache[i], pool_cache[i], ret_cache[i], start_pos)
        x = layer_norm(DEEPNORM_ALPHA * x + m, ln2_g[i], ln2_b[i])
        h = (0.5 * swiglu(x, ffn2_w_gate[i], ffn2_w_up[i], ffn2_w_down[i])
             + jax.nn.sigmoid(x @ ple_gate[i]) * (p[i] @ ple_proj[i]))
        x = layer_norm(DEEPNORM_ALPHA * x + h, ln3_g[i], ln3_b[i])
        convs.append(c_new)
        pools.append(p_new)
        rets.append(r_new)
    return x, jnp.stack(convs), jnp.stack(pools), jnp.stack(rets)


def setup_inputs(seed: int = 0) -> dict:
    key = jax.random.key(seed)
    ks = jax.random.split(key, 32)
    f32 = jnp.float32

    def nrm(k, shape, scale=1.0):
        return jax.random.normal(k, shape, f32) * scale

    D, F = D_MODEL, D_FF
    return {
        "x_prompt": nrm(ks[0], (BATCH, SEQ, D)),
        "x_sample": nrm(ks[1], (DEC_BATCH, DEC_SEQ, D)),
        "p_prompt": nrm(ks[2], (DEPTH, BATCH, SEQ, PLE_DIM)),
        "p_sample": nrm(ks[3], (DEPTH, DEC_BATCH, DEC_SEQ, PLE_DIM)),
        "cache_conv": nrm(ks[4], (DEPTH, DEC_BATCH, CONV_K - 1, CONV_WIDTH)),
        "cache_pool": nrm(ks[5], (DEPTH, DEC_BATCH, POOL_HIST, POOL_WIDTH)),
        "state_ret": nrm(ks[6], (DEPTH, DEC_BATCH, RET_HEADS, RET_HEAD_DIM, RET_HEAD_DIM), RET_HEAD_DIM ** -0.5),
        "ln1_g": 1.0 + nrm(ks[7], (DEPTH, D), 0.02),
        "ln1_b": nrm(ks[8], (DEPTH, D), 0.02),
        "ffn1_w_gate": nrm(ks[9], (DEPTH, D, F), D ** -0.5),
        "ffn1_w_up": nrm(ks[10], (DEPTH, D, F), D ** -0.5),
        "ffn1_w_down": nrm(ks[11], (DEPTH, F, D), F ** -0.5 * DEEPNORM_BETA),
        "w_in": nrm(ks[12], (DEPTH, D, IN_COLS), D ** -0.5),
        "conv_w": nrm(ks[13], (DEPTH, CONV_K, CONV_WIDTH), CONV_K ** -0.5),
        "pool_w": nrm(ks[14], (DEPTH, N_POOL_GROUPS, POOL_GROUP, POOL_GROUP), POOL_GROUP ** -0.5),
        "pool_scale": 1.0 + nrm(ks[15], (DEPTH, POOL_WIDTH), 0.1),
        "w_out": nrm(ks[16], (DEPTH, MIX_WIDTH, D), MIX_WIDTH ** -0.5 * DEEPNORM_BETA),
        "ln2_g": 1.0 + nrm(ks[17], (DEPTH, D), 0.02),
        "ln2_b": nrm(ks[18], (DEPTH, D), 0.02),
        "ffn2_w_gate": nrm(ks[19], (DEPTH, D, F), D ** -0.5),
        "ffn2_w_up": nrm(ks[20], (DEPTH, D, F), D ** -0.5),
        "ffn2_w_down": nrm(ks[21], (DEPTH, F, D), F ** -0.5 * DEEPNORM_BETA),
        "ple_gate": nrm(ks[22], (DEPTH, D, D), D ** -0.5),
        "ple_proj": nrm(ks[23], (DEPTH, PLE_DIM, D), PLE_DIM ** -0.5 * DEEPNORM_BETA),
        "ln3_g": 1.0 + nrm(ks[24], (DEPTH, D), 0.02),
        "ln3_b": nrm(ks[25], (DEPTH, D), 0.02),
    }


def reference(x_prompt, x_sample, p_prompt, p_sample, cache_conv, cache_pool, state_ret,
              ln1_g, ln1_b, ffn1_w_gate, ffn1_w_up, ffn1_w_down, w_in, conv_w, pool_w, pool_scale,
              w_out, ln2_g, ln2_b, ffn2_w_gate, ffn2_w_up, ffn2_w_down, ple_gate, ple_proj, ln3_g, ln3_b):
    Bp = x_prompt.shape[0]
    zero_conv = jnp.zeros((DEPTH, Bp, CONV_K - 1, CONV_WIDTH), x_prompt.dtype)
    zero_pool = jnp.zeros((DEPTH, Bp, POOL_HIST, POOL_WIDTH), x_prompt.dtype)
    zero_ret = jnp.zeros((DEPTH, Bp, RET_HEADS, RET_HEAD_DIM, RET_HEAD_DIM), jnp.float32)
    y_prompt, conv_p, pool_p, ret_p = trunk(
        x_prompt, p_prompt, zero_conv, zero_pool, zero_ret, 0,
        ln1_g, ln1_b, ffn1_w_gate, ffn1_w_up, ffn1_w_down, w_in, conv_w, pool_w, pool_scale,
        w_out, ln2_g, ln2_b, ffn2_w_gate, ffn2_w_up, ffn2_w_down, ple_gate, ple_proj, ln3_g, ln3_b)
    y_sample, conv_s, pool_s, ret_s = trunk(
        x_sample, p_sample, cache_conv, cache_pool, state_ret, PAST_LEN,
        ln1_g, ln1_b, ffn1_w_gate, ffn1_w_up, ffn1_w_down, w_in, conv_w, pool_w, pool_scale,
        w_out, ln2_g, ln2_b, ffn2_w_gate, ffn2_w_up, ffn2_w_down, ple_gate, ple_proj, ln3_g, ln3_b)
    return (y_prompt, y_sample, conv_p, pool_p, ret_p, conv_s, pool_s, ret_s)
```

```python
import math
import os
import contextlib
import numpy as np
import concourse.bass as bass
import concourse.mybir as mybir
from concourse.bass_utils import run_bass_kernel_spmd

F32 = mybir.dt.float32
BF16 = mybir.dt.bfloat16
AF = mybir.ActivationFunctionType
ALU = mybir.AluOpType

D = 2048
FF = 5632
NL = 2
T = 1152
TPR = 1024
NTT = 3
TW = 384
NCH = 9
ALPHA = 4 ** 0.25
EPS = 1e-5
GAM = [1.0 - 2.0 ** (-5 - h) for h in range(4)]
NSLOT = 4
ARENA = 64512
NV = 224
TC_COS, TC_SIN, TC_MP, TC_MS, TC_QP, TC_QS = 0, 1152, 2304, 2816, 3328, 3840
TC_K1, TC_K2, TC_KS, TC_OH, TC_ISB, TC_CORR, TC_ID, TC_ONES = 4352, 4384, 4388, 4392, 4408, 4416, 4480, 4608
NTAB = 4736


class _Op:
    __slots__ = ("eng", "fn", "deps", "is_dma", "dma_sem", "dma_val", "signal", "count", "idx", "inc", "near")


SMALL_KEYS = {"stat", "stat3", "xs1", "xr1", "cvt", "pvt", "mean", "rstd", "corr"}


class Prog:
    ENGS = ("pe", "act", "dve", "pool", "sp")

    def __init__(self, nc):
        self.nc = nc
        self.ops = []
        self.last_w = {}
        self.readers = {}
        self.dma_sems = {}
        self.fence_op = None
        self.pe_n = 0
        self.marks = []

    def _add(self, eng, fn, reads, writes, is_dma, dma_sem, inc=16):
        op = _Op()
        op.eng, op.fn, op.is_dma, op.dma_sem = eng, fn, is_dma, dma_sem
        op.signal = False
        op.count = 0
        op.inc = inc
        op.idx = len(self.ops)
        op.near = any(isinstance(k, tuple) and len(k) > 1 and k[0] == "A" and k[1] in SMALL_KEYS
                      for k in list(reads) + list(writes))
        deps = set()
        for k in list(reads) + list(writes):
            if (self.fence_op is not None and isinstance(k, tuple) and k and k[0] == "A"
                    and k not in self.last_w and k not in self.readers):
                deps.add(self.fence_op)
        for r in reads:
            w = self.last_w.get(r)
            if w is not None:
                deps.add(w)
        for w_ in writes:
            w = self.last_w.get(w_)
            if w is not None:
                deps.add(w)
            for rd in self.readers.get(w_, ()):
                deps.add(rd)
        for r in reads:
            if isinstance(r, tuple) and r and r[0] in ("ps", "ps7"):
                for rd in self.readers.get(r, ()):
                    deps.add(rd)
        op.deps = deps
        for r in reads:
            self.readers.setdefault(r, []).append(op.idx)
        for w_ in writes:
            self.last_w[w_] = op.idx
            self.readers[w_] = []
        if is_dma:
            ent = self.dma_sems.setdefault(dma_sem, [None, 0])
            ent[1] += inc
            op.dma_val = ent[1]
        self.ops.append(op)
        return op

    def op(self, eng, fn, reads=(), writes=(), n=1):
        if eng == "pe":
            self.pe_n += n
        return self._add(eng, fn, reads, writes, False, None)

    def mark(self, label):
        self.marks.append((label, self.pe_n))

    def dma(self, eng, fn, sem, reads=(), writes=(), inc=16):
        return self._add(eng, fn, reads, writes, True, sem, inc)

    def fence(self, fn):
        akeys = [k for k in set(list(self.last_w) + list(self.readers))
                 if isinstance(k, tuple) and k and k[0] == "A"]
        op = self._add("dve", fn, akeys, akeys, False, None)
        for k in akeys:
            self.last_w.pop(k, None)
            self.readers.pop(k, None)
        self.fence_op = op.idx

    def emit(self):
        nc = self.nc
        ops = self.ops
        epos = {e: 0 for e in self.ENGS}
        for op in ops:
            epos[op.eng] += 1
            op.count = epos[op.eng]
        pos = {op.idx: op.count for op in ops}
        self._pos = pos
        NEAR = 6

        def same_eng_skip(op, dop):
            if dop.eng != op.eng or op.is_dma:
                return False
            if (op.near or dop.near) and op.eng in ("act", "dve", "pool") and pos[op.idx] - pos[dop.idx] <= NEAR:
                return False
            return True
        self._skip = same_eng_skip
        for op in ops:
            for d in op.deps:
                dop = ops[d]
                if dop.is_dma:
                    continue
                if same_eng_skip(op, dop):
                    continue
                dop.signal = True
        cnt = {e: 0 for e in self.ENGS}
        for op in ops:
            if op.signal:
                cnt[op.eng] += 1
            op.count = cnt[op.eng]
        by_eng = {e: [o for o in ops if o.eng == e] for e in self.ENGS}
        with contextlib.ExitStack() as st:
            esem = {e: st.enter_context(nc.semaphore("sem_" + e)) for e in self.ENGS}
            for i, (name, ent) in enumerate(self.dma_sems.items()):
                ent[0] = st.enter_context(nc.semaphore("dsem_%d" % i))
            block = st.enter_context(nc.Block())
            dma_final = [(ent[0], ent[1]) for ent in self.dma_sems.values()]

            def run(ename, e):
                waited = {}
                for op in by_eng[ename]:
                    need = {}
                    for d in op.deps:
                        dop = ops[d]
                        if dop.is_dma:
                            key = ("d", dop.dma_sem)
                            sem, val = self.dma_sems[dop.dma_sem][0], dop.dma_val
                        else:
                            if self._skip(op, dop):
                                continue
                            key = ("e", dop.eng)
                            sem, val = esem[dop.eng], dop.count
                        if need.get(key, (None, -1))[1] < val:
                            need[key] = (sem, val)
                    for key, (sem, val) in need.items():
                        if waited.get(key, -1) >= val:
                            continue
                        e.wait_ge(sem, val)
                        waited[key] = val
                    ins = op.fn(e)
                    if op.is_dma:
                        ins.then_inc(self.dma_sems[op.dma_sem][0], op.inc)
                    elif op.signal:
                        ins.then_inc(esem[ename], 1)
                if ename == "sp":
                    for sem, val in dma_final:
                        e.wait_ge(sem, val)
                    for en in self.ENGS:
                        if en != "sp" and cnt[en] > 0:
                            e.wait_ge(esem[en], cnt[en])

            @block.tensor
            def _(e):
                run("pe", e)

            @block.scalar
            def _(e):
                run("act", e)

            @block.vector
            def _(e):
                run("dve", e)

            @block.gpsimd
            def _(e):
                run("pool", e)

            @block.sync
            def _(e):
                run("sp", e)


class Buf:
    def __init__(self, ap2d, off=0):
        self.t = ap2d.tensor
        self.base = ap2d.offset + off
        self.ps = ap2d.ap[0][0]

    def __call__(self, off, *dims, p0=0, pn=128):
        return bass.AP(self.t, self.base + p0 * self.ps + off, [[self.ps, pn]] + [list(d) for d in dims])


_NC_CACHE = {}


def build_program():
    nc = bass.Bass("TRN2", target_bir_lowering=False)
    dt_in = lambda n, s: nc.dram_tensor(n, s, F32, kind="ExternalInput")
    dt_out = lambda n, s: nc.dram_tensor(n, s, F32, kind="ExternalOutput")
    ISH = {"x": [T, D], "p": [NL, T, 256], "cconv": [NL, 32, 512], "cpool": [NL, 240, 512],
           "sret": [NL, 16, 4, 256, 256], "vecs": [128, NV], "tabs": [128, NTAB]}

    class _LazyI(dict):
        def __missing__(self, nm):
            self[nm] = dt_in(nm, ISH[nm])
            return self[nm]
    I = _LazyI()

    class _L:
        def __init__(self, nm):
            self.nm = nm

        def ap(self):
            return I[self.nm].ap()
    x_d, p_d, cconv_d, cpool_d, sret_d, vecs_d, tabs_d = (_L(n) for n in ("x", "p", "cconv", "cpool", "sret", "vecs", "tabs"))
    WSH = {"ffn1_w_gate": [NL, D, FF], "ffn1_w_up": [NL, D, FF], "ffn1_w_down": [NL, FF, D],
           "w_in": [NL, D, 6144], "pool_w": [NL, 4, 128, 128], "w_out": [NL, D, D],
           "ffn2_w_gate": [NL, D, FF], "ffn2_w_up": [NL, D, FF], "ffn2_w_down": [NL, FF, D],
           "ple_gate": [NL, D, D], "ple_proj": [NL, 256, D]}

    class _LazyW(dict):
        def __missing__(self, nm):
            self[nm] = dt_in(nm, WSH[nm])
            return self[nm]
    W = _LazyW()
    OSH = {"y": [T, D], "oconv_s": [NL, 32, 512], "opool_s": [NL, 240, 512], "oret_s": [NL, 16, 4, 256, 256],
           "oconv_p": [NL, 2, 512], "opool_p": [NL, 15, 512], "oret_p": [NL, 4, 256, 256]}

    class _LazyO(dict):
        def __missing__(self, nm):
            self[nm] = dt_out(nm, OSH[nm])
            return self[nm]
    O = _LazyO()

    class _LO:
        def __init__(self, nm):
            self.nm = nm

        def ap(self):
            return O[self.nm].ap()
    y_d, oconv_s, opool_s, oret_s, oconv_p, opool_p, oret_p = (
        _LO(n) for n in ("y", "oconv_s", "opool_s", "oret_s", "oconv_p", "opool_p", "oret_p"))

    class _LazyC(dict):
        def __missing__(self, key):
            nm, l = key
            shp = {"cc1_in": [128, 72], "cc1_out": [256, 72], "cc2_in": [128, 2048], "cc2_out": [256, 2048]}[nm]
            self[key] = nc.dram_tensor("%s%d" % (nm, l), shp, F32)
            return self[key]
    CC = _LazyC()

    class _LC:
        def __init__(self, nm):
            self.nm = nm

        def __getitem__(self, l):
            return CC[(self.nm, l)]
    cc1_in, cc1_out, cc2_in, cc2_out = _LC("cc1_in"), _LC("cc1_out"), _LC("cc2_in"), _LC("cc2_out")
    PAIRS = [[0, 1], [2, 3], [4, 5], [6, 7]]

    P = Prog(nc)
    stage = int(os.environ.get("KSTAGE", "999"))

    class _Stop(Exception):
        pass

    def chk(n):
        P.mark("stage%d" % n)
        if n > stage:
            raise _Stop()

    with contextlib.ExitStack() as st:
        sbt = lambda n, s, d: st.enter_context(nc.sbuf_tensor("sb_" + n, s, d))
        xres_t = sbt("xres", [128, 16 * T], F32)
        xbf_t = sbt("xbf", [128, 16 * T], BF16)
        tabs_t = sbt("tabs", [128, NTAB], F32)
        vecs_t = sbt("vecs", [128, NV], F32)
        valp_t = sbt("valp", [128, NV], F32)
        idb_t = sbt("idb", [128, 128], BF16)
        wsl_t = sbt("wsl", [128, NSLOT * 2048], BF16)
        ar_t = sbt("arena", [128, ARENA // 4], F32)
        psb = [st.enter_context(nc.psum_tensor("ps%d" % i, [128, 512], F32)) for i in range(6)]
        ps7_t = [st.enter_context(nc.psum_tensor("psb%d" % i, [128, 1024], BF16)) for i in range(2)]

        XR = Buf(xres_t[:, :])
        XB = Buf(xbf_t[:, :])
        TB = Buf(tabs_t[:, :])
        VC = Buf(vecs_t[:, :])
        VA = Buf(valp_t[:, :])
        IDB = Buf(idb_t[:, :])
        WS = Buf(wsl_t[:, :])
        AF32 = Buf(ar_t[:, :])
        ABF = Buf(ar_t[:, :].bitcast(BF16))
        PS = [Buf(b[:, :]) for b in psb]
        PS7L = [Buf(t_[:, :]) for t_ in ps7_t]

        def af(off_b):
            return Buf(ar_t[:, :], off_b // 4)

        def ab(off_b):
            return Buf(ar_t[:, :].bitcast(BF16), off_b // 2)

        IDF = TB(TC_ID, (1, 128))
        ONES = TB(TC_ONES, (1, 128))

        bank_ctr = [0]

        def nbank():
            b = bank_ctr[0] % 4
            bank_ctr[0] += 1
            return b

        misc_ctr = [0]

        def mbank():
            b = 4 + misc_ctr[0] % 2
            misc_ctr[0] += 1
            return b

        h7_ctr = [0]

        def h7():
            b = h7_ctr[0] % 2
            h7_ctr[0] += 1
            return b

        def mm(groups, reads, writes):
            def fn(e):
                ins = None
                for out, pairs in groups:
                    n = len(pairs)
                    for i, (l, r) in enumerate(pairs):
                        ins = e.matmul(out, l, r, start=(i == 0), stop=(i == n - 1))
                return ins
            P.op("pe", fn, reads, writes, n=sum(len(p_) for _, p_ in groups))

        def tr(items, reads, writes):
            def fn(e):
                ins = None
                for o, i_, idn in items:
                    ins = e.transpose(o, i_, idn)
                return ins
            P.op("pe", fn, reads, writes, n=len(items))

        def act(out, in_, func, reads, writes, bias=None, scale=None):
            kw = {}
            if bias is not None:
                kw["bias"] = bias
            if scale is not None:
                kw["scale"] = scale
            P.op("act", lambda e: e.activation(out, in_, func, **kw), reads, writes)

        def tt(out, in0, in1, op, reads, writes, eng="dve"):
            P.op(eng, lambda e: e.tensor_tensor(out, in0, in1, op), reads, writes)

        def ts(out, in0, s1, s2, op0, op1, reads, writes, eng="dve"):
            if s2 is None:
                P.op(eng, lambda e: e.tensor_scalar(out, in0, s1, None, op0), reads, writes)
            else:
                P.op(eng, lambda e: e.tensor_scalar(out, in0, s1, s2, op0, op1), reads, writes)

        def stt(out, in0, sc, in1, op0, op1, reads, writes, eng="dve"):
            P.op(eng, lambda e: e.scalar_tensor_tensor(out, in0, sc, in1, op0, op1), reads, writes)

        def cp(out, in_, reads, writes, eng="dve"):
            if eng == "act":
                P.op(eng, lambda e: e.activation(out, in_, AF.Copy), reads, writes)
            else:
                P.op(eng, lambda e: e.tensor_copy(out, in_), reads, writes)

        sp_ctr = [0]

        def spdma(out, in_, reads, writes, sem=None):
            if sem is None:
                sem = ("sp", sp_ctr[0] % 8)
                sp_ctr[0] += 1
            P.dma("sp", lambda e: e.dma_start(out=out, in_=in_), sem, reads, writes)

        ws_ctr = [0]

        def wload(dram_ap, nk):
            s = ws_ctr[0] % NSLOT
            ws_ctr[0] += 1
            key = ("w", s)
            out = WS(s * 2048, (128, nk), (1, 128))
            P.dma("pool", lambda e: e.dma_start(out=out, in_=dram_ap), ("w", s), (), [key])
            return (lambda kc, s=s: WS(s * 2048 + kc * 128, (1, 128))), key

        def wtile(name, l, r0, nk, c0):
            return W[name].ap()[l][r0:r0 + nk * 128, c0:c0 + 128].rearrange("(k p) c -> p k c", p=128)

        def xb_rhs(kc, t0, n):
            return XB(kc * T + t0, (1, n))

        KX = lambda t: ("xbf", t)
        KR = lambda t: ("xres", t)

        spdma(tabs_t[:, :], tabs_d.ap(), (), ["tabs"], sem="c0")
        spdma(vecs_t[:, :], vecs_d.ap(), (), ["vecs"], sem="c1")
        KSUB = int(os.environ.get("KSUB", "0"))
        if not KSUB & 1:
            ts(valp_t[:, :], vecs_t[:, :], float(ALPHA), None, ALU.mult, None, ["vecs"], ["valp"])
        if not KSUB & 8:
            cp(idb_t[:, :], IDF, ["tabs"], ["idb"])

        for j in range(NCH if not KSUB & 2 else 0):
            sl = j % 2
            xin = af(sl * 8192)
            kx = ("A", "xin", sl)
            spdma(xin(0, (1, 2048)), x_d.ap()[j * 128:(j + 1) * 128, :], (), [kx], sem=("xin", sl))
            for q in range(4):
                b = mbank()
                tr([(PS[b](i * 128, (1, 128)), xin((q * 4 + i) * 128, (1, 128)), IDF) for i in range(4)],
                   [kx, "tabs"], [("ps", b)])
                o_r = XR(q * 4 * T + j * 128, (T, 4), (1, 128))
                o_b = XB(q * 4 * T + j * 128, (T, 4), (1, 128))
                src = PS[b](0, (128, 4), (1, 128))
                if not KSUB & 16:
                    act(o_r, src, AF.Identity, [("ps", b)], [KR(j // 3)], scale=float(ALPHA))
                if not KSUB & 32:
                    cp(o_b, src, [("ps", b)], [KX(j // 3)])

        fence_nop = lambda e: e.tensor_copy(valp_t[:, 0:1], valp_t[:, 0:1])

        def ffn(l, pre):
            P.fence(lambda e: e.tensor_copy(valp_t[:, 0:1], valp_t[:, 0:1]))
            hb = ab(0)
            sg = af(25344)
            sgc = [0]
            for g in range(4):
                for fi in range(11):
                    ft = g * 11 + fi
                    wg, kg = wload(wtile(pre + "_w_gate", l, 0, 16, ft * 128), 16)
                    wu, ku = wload(wtile(pre + "_w_up", l, 0, 16, ft * 128), 16)
                    for t in range(NTT):
                        ba, bb = nbank(), nbank()
                        mm([(PS[ba](0, (1, TW)), [(wg(kc), xb_rhs(kc, t * TW, TW)) for kc in range(16)])],
                           [kg, KX(t)], [("ps", ba)])
                        mm([(PS[bb](0, (1, TW)), [(wu(kc), xb_rhs(kc, t * TW, TW)) for kc in range(16)])],
                           [ku, KX(t)], [("ps", bb)])
                        s_ = sgc[0] % 3
                        sgc[0] += 1
                        ksg = ("A", "sg", s_)
                        act(sg(s_ * TW, (1, TW)), PS[ba](0, (1, TW)), AF.Silu, [("ps", ba)], [ksg])
                        tt(hb(fi * T + t * TW, (1, TW)), sg(s_ * TW, (1, TW)), PS[bb](0, (1, TW)), ALU.mult,
                           [ksg, ("ps", bb)], [("A", "h", t)])
                def down(dc, t, wd, kd):
                    b = nbank()
                    mm([(PS[b](0, (1, TW)), [(wd(fi), hb(fi * T + t * TW, (1, TW))) for fi in range(11)])],
                       [kd, ("A", "h", t)], [("ps", b)])
                    xr = XR(dc * T + t * TW, (1, TW))
                    stt(xr, PS[b](0, (1, TW)), 0.5, xr, ALU.mult, ALU.add, [("ps", b), KR(t)], [KR(t)])
                if True:
                    for dc in range(16):
                        wd, kd = wload(wtile(pre + "_w_down", l, g * 1408, 11, dc * 128), 11)
                        for t in range(NTT):
                            down(dc, t, wd, kd)
                else:
                    for t in range(NTT):
                        for dc in range(16):
                            wd, kd = wload(wtile(pre + "_w_down", l, g * 1408, 11, dc * 128), 11)
                            down(dc, t, wd, kd)

        def layernorm(l, which, final=False):
            gcol = l * 112 + which * 32
            bcol = gcol + 16
            P.fence(fence_nop)
            mean = af(30208)
            rstd = af(30208 + 1536)
            sq = af(30208 + 3072)
            for t in range(NTT):
                kt = KR(t)
                b = mbank()
                mm([(PS[b](0, (1, TW)), [(ONES, XR(dc * T + t * TW, (1, TW))) for dc in range(16)])],
                   [kt, "tabs"], [("ps", b)])
                cp(mean(0, (1, TW)), PS[b](0, (1, TW)), [("ps", b)], [("A", "mean")])
                xr3 = XR(t * TW, (T, 16), (1, TW))
                tt(xr3, xr3, mean(0, (0, 16), (1, TW)), ALU.subtract, [kt, ("A", "mean")], [kt])
                b2 = mbank()
                for dc in range(16):
                    s_ = dc % 2
                    act(sq(s_ * TW, (1, TW)), XR(dc * T + t * TW, (1, TW)), AF.Square, [kt], [("A", "sq", s_)])
                    o = PS[b2](0, (1, TW))
                    l_, r_ = ONES, sq(s_ * TW, (1, TW))
                    P.op("pe", (lambda e, o=o, l_=l_, r_=r_, dc=dc: e.matmul(o, l_, r_, start=(dc == 0), stop=(dc == 15))),
                         [("A", "sq", s_), "tabs"], [("ps", b2)])
                act(rstd(0, (1, TW)), PS[b2](0, (1, TW)), AF.Sqrt, [("ps", b2)], [("A", "rstd")], bias=float(EPS))
                P.op("dve", lambda e: e.reciprocal(rstd(0, (1, TW)), rstd(0, (1, TW))), [("A", "rstd")], [("A", "rstd")])
                tt(xr3, xr3, rstd(0, (0, 16), (1, TW)), ALU.mult, [kt, ("A", "rstd")], [kt])
                for dc in range(16):
                    xr = XR(dc * T + t * TW, (1, TW))
                    if not final:
                        act(XB(dc * T + t * TW, (1, TW)), xr, AF.Identity, [kt, "vecs"], [KX(t)],
                            bias=VC(bcol + dc, (1, 1)), scale=VC(gcol + dc, (1, 1)))
                        act(xr, xr, AF.Identity, [kt, "valp"], [kt],
                            bias=VA(bcol + dc, (1, 1)), scale=VA(gcol + dc, (1, 1)))
                    else:
                        act(xr, xr, AF.Identity, [kt, "vecs"], [kt],
                            bias=VC(bcol + dc, (1, 1)), scale=VC(gcol + dc, (1, 1)))

        def wout_group(l, r0, nk, mixbuf, mixkey, tt_outer=False):
            def one(dc, t, w, kw):
                b = nbank()
                mm([(PS[b](0, (1, TW)), [(w(kc), mixbuf(kc * T + t * TW, (1, TW))) for kc in range(nk)])],
                   [kw, mixkey], [("ps", b)])
                xr = XR(dc * T + t * TW, (1, TW))
                tt(xr, PS[b](0, (1, TW)), xr, ALU.add, [("ps", b), KR(t)], [KR(t)])
            if not tt_outer:
                for dc in range(16):
                    w, kw = wload(wtile("w_out", l, r0, nk, dc * 128), nk)
                    for t in range(NTT):
                        one(dc, t, w, kw)
            else:
                for t in range(NTT):
                    for dc in range(16):
                        w, kw = wload(wtile("w_out", l, r0, nk, dc * 128), nk)
                        one(dc, t, w, kw)

        def zmm(l, col0, evac):
            w, kw = wload(wtile("w_in", l, 0, 16, col0), 16)
            for t in range(NTT):
                b = nbank()
                mm([(PS[b](0, (1, TW)), [(w(kc), xb_rhs(kc, t * TW, TW)) for kc in range(16)])],
                   [kw, KX(t)], [("ps", b)])
                evac(t, b)

        def pair_mm(l, colA, colB, evac):
            w1, k1 = wload(wtile("w_in", l, 0, 16, colA), 16)
            w2, k2 = wload(wtile("w_in", l, 0, 16, colB), 16)
            for t in range(NTT):
                b1, b2 = nbank(), nbank()
                mm([(PS[b1](0, (1, TW)), [(w1(kc), xb_rhs(kc, t * TW, TW)) for kc in range(16)])],
                   [k1, KX(t)], [("ps", b1)])
                mm([(PS[b2](0, (1, TW)), [(w2(kc), xb_rhs(kc, t * TW, TW)) for kc in range(16)])],
                   [k2, KX(t)], [("ps", b2)])
                evac(t, b1, b2)

        def rot_pair(l, col0, outbuf, outkey, rt):
            def ev(t, b1, b2):
                cs = TB(TC_COS + t * TW, (1, TW))
                sn = TB(TC_SIN + t * TW, (1, TW))
                p1, p2 = PS[b1](0, (1, TW)), PS[b2](0, (1, TW))
                t1, t2 = rt(0, (1, TW)), rt(TW, (1, TW))
                ka, kb = ("A", "rt", 0), ("A", "rt", 1)
                tt(t1, p1, cs, ALU.mult, [("ps", b1), "tabs"], [ka])
                tt(t2, p2, sn, ALU.mult, [("ps", b2), "tabs"], [kb])
                tt(outbuf(t * TW, (1, TW)), t1, t2, ALU.subtract, [ka, kb], [outkey])
                tt(t1, p1, sn, ALU.mult, [("ps", b1), "tabs"], [ka])
                tt(t2, p2, cs, ALU.mult, [("ps", b2), "tabs"], [kb])
                tt(outbuf(T + t * TW, (1, TW)), t1, t2, ALU.add, [ka, kb], [outkey])
            pair_mm(l, col0, col0 + 128, ev)

        def mixing(l):
            P.fence(fence_nop)
            UW, PW = 1186, 1407
            uf = af(0)
            zp = af(18976)
            zc = af(41488)
            mixb = ab(46096)
            cst = af(55312)
            xs1 = af(59408)
            KU = lambda ci: ("A", "uf", ci)
            KZ = lambda gi: ("A", "zp", gi)
            vb = l * 112

            spdma(cst(0, (1, 512), pn=32), cconv_d.ap()[l], (), [("A", "cst")], sem="cst")
            for ci in range(4):
                b = mbank()
                tr([(PS[b](0, (1, 32)), cst(ci * 128, (1, 128), pn=32), TB(TC_ID, (1, 32), pn=32))],
                   [("A", "cst"), "tabs"], [("ps", b)])
                cp(uf(ci * UW + 1026, (10, 16), (1, 2)), PS[b](0, (2, 16), (1, 2)), [("ps", b)], [KU(ci)])
            for half, (r0, nr) in enumerate(((0, 128), (128, 112))):
                spdma(cst(0, (1, 512), pn=nr), cpool_d.ap()[l][r0:r0 + nr, :], (), [("A", "cst")], sem="cst")
                for gi in range(4):
                    b = mbank()
                    tr([(PS[b](0, (1, nr)), cst(gi * 128, (1, 128), pn=nr), TB(TC_ID, (1, nr), pn=nr))],
                       [("A", "cst"), "tabs"], [("ps", b)])
                    if half == 0:
                        cp(zp(gi * PW + 1039, (23, 8), (1, 15)), PS[b](0, (15, 8), (1, 15)), [("ps", b)], [KZ(gi)])
                        cp(zp(gi * PW + 1039 + 8 * 23, (1, 8)), PS[b](120, (1, 8)), [("ps", b)], [KZ(gi)])
                    else:
                        cp(zp(gi * PW + 1039 + 8 * 23 + 8, (1, 7)), PS[b](0, (1, 7)), [("ps", b)], [KZ(gi)])
                        cp(zp(gi * PW + 1039 + 9 * 23, (23, 7), (1, 15)), PS[b](7, (15, 7), (1, 15)),
                           [("ps", b)], [KZ(gi)])

            for ci in range(4):
                def ev_c(t, b):
                    act(zc(t * TW, (1, TW)), PS[b](0, (1, TW)), AF.Copy, [("ps", b)], [("A", "zc")])
                zmm(l, 512 + ci * 128, ev_c)

                def ev_h(t, b, ci=ci):
                    if t < 2:
                        tt(uf(ci * UW + 2 + t * TW, (1, TW)), PS[b](0, (1, TW)), zc(t * TW, (1, TW)), ALU.mult,
                           [("ps", b), ("A", "zc")], [KU(ci)])
                    else:
                        tt(uf(ci * UW + 2 + 768, (1, 256)), PS[b](0, (1, 256)), zc(768, (1, 256)), ALU.mult,
                           [("ps", b), ("A", "zc")], [KU(ci)])
                        tt(uf(ci * UW + 1028, (10, 16), (1, 8)), PS[b](256, (8, 16), (1, 8)),
                           zc(1024, (8, 16), (1, 8)), ALU.mult, [("ps", b), ("A", "zc")], [KU(ci)])
                zmm(l, 1024 + ci * 128, ev_h)
            for gi in range(4):
                def ev_p(t, b, gi=gi):
                    if t < 2:
                        act(zp(gi * PW + 15 + t * TW, (1, TW)), PS[b](0, (1, TW)), AF.Copy, [("ps", b)], [KZ(gi)])
                    else:
                        act(zp(gi * PW + 15 + 768, (1, 256)), PS[b](0, (1, 256)), AF.Copy, [("ps", b)], [KZ(gi)])
                        act(zp(gi * PW + 1039 + 15, (23, 16), (1, 8)), PS[b](256, (8, 16), (1, 8)), AF.Copy,
                            [("ps", b)], [KZ(gi)])
                zmm(l, 1536 + gi * 128, ev_p)

            chk(4 + 20 * l)
            allu = [KU(i) for i in range(4)]
            allz = [KZ(i) for i in range(4)]
            cp(xs1(0, (2, 4), (1, 2)), uf(1024, (UW, 4), (1, 2)), allu, [("A", "xs1")])
            cp(xs1(8, (15, 4), (1, 15)), zp(1024, (PW, 4), (1, 15)), allz, [("A", "xs1")])
            P.op("dve", lambda e: e.memset(xs1(68, (1, 4)), 0.0), [], [("A", "xs1")])
            P.dma("pool", lambda e: e.dma_start(out=cc1_in[l].ap(), in_=xs1(0, (1, 72))), ("cc1i", l),
                  [("A", "xs1")], [("cc1in", l)])
            NOCC = bool(int(os.environ.get("KSUB", "0")) & 64)
            if not NOCC:
                P.dma("pool", lambda e: e.collective_compute("AllGather", ALU.bypass, replica_groups=PAIRS,
                                                             ins=[cc1_in[l].ap().opt()], outs=[cc1_out[l].ap().opt()]),
                      ("cc1", l), [("cc1in", l)], [("cc1out", l)], inc=1)
                P.dma("pool", lambda e: e.dma_start(out=xs1(72, (1, 72)), in_=cc1_out[l].ap()[0:128, :]), ("cc1o", l),
                      [("cc1out", l)], [("A", "xr1")])
            else:
                P.dma("pool", lambda e: e.dma_start(out=xs1(72, (1, 72)), in_=cc1_in[l].ap()), ("cc1o", l),
                      [("cc1in", l)], [("A", "xr1")])
            isb = TB(TC_ISB, (1, 1))
            ts(uf(0, (UW, 4), (1, 2)), xs1(72, (2, 4), (1, 2)), isb, None, ALU.mult, None,
               [("A", "xr1"), "tabs"], allu)
            ts(zp(0, (PW, 4), (1, 15)), xs1(80, (15, 4), (1, 15)), isb, None, ALU.mult, None,
               [("A", "xr1"), "tabs"], allz)

            chk(5 + 20 * l)
            for ci in range(4):
                wc = lambda j, ci=ci: VC(vb + 96 + ci * 3 + j, (1, 1))
                kk = [KU(ci), "vecs"]
                ts(zc(0, (1, 1024)), uf(ci * UW, (1, 1024)), wc(0), None, ALU.mult, None, kk, [("A", "zc")])
                ts(zc(1024, (8, 16), (1, 8)), uf(ci * UW + 1026, (10, 16), (1, 8)), wc(0), None, ALU.mult, None,
                   kk, [("A", "zc")])
                for j in (1, 2):
                    stt(zc(0, (1, 1024)), uf(ci * UW + j, (1, 1024)), wc(j), zc(0, (1, 1024)), ALU.mult, ALU.add,
                        kk + [("A", "zc")], [("A", "zc")])
                    stt(zc(1024, (8, 16), (1, 8)), uf(ci * UW + 1026 + j, (10, 16), (1, 8)), wc(j),
                        zc(1024, (8, 16), (1, 8)), ALU.mult, ALU.add, kk + [("A", "zc")], [("A", "zc")])

                def ev_b(t, b, ci=ci):
                    tt(mixb(ci * T + t * TW, (1, TW)), PS[b](0, (1, TW)), zc(t * TW, (1, TW)), ALU.mult,
                       [("ps", b), ("A", "zc")], [("A", "mix")])
                zmm(l, ci * 128, ev_b)
            cvt = af(59408 + 576)
            cp(cvt(0, (34, 4), (2, 16), (1, 2)), uf(1026 + 8, (UW, 4), (10, 16), (1, 2)), allu, [("A", "cvt")])
            cp(cvt(32, (34, 4), (1, 2)), uf(1024, (UW, 4), (1, 2)), allu, [("A", "cvt")])
            b = mbank()
            tr([(PS[b](ci * 128, (1, 128), pn=34), cvt(ci * 34, (1, 34)), IDF) for ci in range(4)],
               [("A", "cvt"), "tabs"], [("ps", b)])
            cp(cst(0, (1, 512), pn=34), PS[b](0, (1, 512), pn=34), [("ps", b)], [("A", "cst")])
            spdma(oconv_s.ap()[l], cst(0, (1, 512), pn=32), [("A", "cst")], [], sem="cst")
            spdma(oconv_p.ap()[l], cst(0, (1, 512), p0=32, pn=2), [("A", "cst")], [], sem="cst")
            wout_group(l, 0, 4, mixb, ("A", "mix"))

            chk(6 + 20 * l)
            pa = af(0)
            pb_ = af(5632)
            dT = ab(11264)
            P.fence(fence_nop)
            wpl, kpl = wload(W["pool_w"].ap()[l].rearrange("g c d -> c g d"), 4)
            pvt = af(13568)
            for gi in range(4):
                wdw = 2 << gi
                src = lambda off, n, gi=gi: zp(gi * PW + off, (1, n))
                src3 = lambda off, n, gi=gi: zp(gi * PW + 1039 + off, (23, 16), (1, n))
                cur, cur3 = src, src3
                bufs = [pa, pb_]
                sh = 1
                step = 0
                lo = 0
                while sh < wdw:
                    ob = bufs[step % 2]
                    kin = [KZ(gi), ("A", "pp", 0), ("A", "pp", 1)]
                    lo2 = lo + sh
                    tt(ob(lo2, (1, 1039 - lo2)), cur(lo2, 1039 - lo2), cur(lo2 - sh, 1039 - lo2), ALU.add,
                       kin, [("A", "pp", step % 2)])
                    tt(ob(1039 + lo2, (23, 16), (1, 23 - lo2)), cur3(lo2, 23 - lo2), cur3(lo2 - sh, 23 - lo2), ALU.add,
                       kin, [("A", "pp", step % 2)])
                    cur = lambda off, n, ob=ob: ob(off, (1, n))
                    cur3 = lambda off, n, ob=ob: ob(1039 + off, (23, 16), (1, n))
                    lo = lo2
                    sh *= 2
                    step += 1
                kin = [KZ(gi), ("A", "pp", 0), ("A", "pp", 1), "tabs"]
                last = ("A", "pp", (step - 1) % 2)
                tt(cur(15, 16), cur(15, 16), TB(TC_CORR + gi * 16, (1, 16)), ALU.mult, kin, [last, ("A", "corr")])
                stt(dT(0, (1, 1024)), cur(15, 1024), 1.0 / wdw, src(15, 1024), ALU.mult, ALU.subtract,
                    kin, [("A", "dT")])
                stt(dT(1024, (8, 16), (1, 8)), cur3(15, 8), 1.0 / wdw, src3(15, 8), ALU.mult, ALU.subtract,
                    kin, [("A", "dT")])
                for t in range(NTT):
                    b = nbank()
                    mm([(PS[b](0, (1, TW)), [(wpl(gi), dT(t * TW, (1, TW)))])], [kpl, ("A", "dT")], [("ps", b)])
                    act(mixb(gi * T + t * TW, (1, TW)), PS[b](0, (1, TW)), AF.Identity, [("ps", b), "vecs"],
                        [("A", "mix")], scale=VC(vb + 108 + gi, (1, 1)))
                cp(pvt(gi * 255, (15, 16), (1, 15)), zp(gi * PW + 1039 + 8, (23, 16), (1, 15)), [KZ(gi)], [("A", "pvt")])
                cp(pvt(gi * 255 + 240, (1, 15)), zp(gi * PW + 1024, (1, 15)), [KZ(gi)], [("A", "pvt")])
            pst = af(55312)
            for blk, (c0, nr) in enumerate(((0, 128), (128, 127))):
                b = mbank()
                tr([(PS[b](gi * 128, (1, 128), pn=nr), pvt(gi * 255 + c0, (1, nr)), IDF) for gi in range(4)],
                   [("A", "pvt"), "tabs"], [("ps", b)])
                cp(pst(blk * 512, (1, 512), pn=nr), PS[b](0, (1, 512), pn=nr), [("ps", b)], [("A", "cst")])
            spdma(opool_s.ap()[l][0:128, :], pst(0, (1, 512)), [("A", "cst")], [], sem="cst")
            spdma(opool_s.ap()[l][128:240, :], pst(512, (1, 512), pn=112), [("A", "cst")], [], sem="cst")
            spdma(opool_p.ap()[l], pst(512, (1, 512), p0=112, pn=15), [("A", "cst")], [], sem="cst")
            wout_group(l, 512, 4, mixb, ("A", "mix"))

            chk(7 + 20 * l)
            P.fence(fence_nop)
            kT = ab(0)
            qT = ab(4608)
            kdt = ab(9216)
            vtm = ab(13824)
            sgt = ab(18432)
            mixc = ab(23040)
            rt = af(27648)
            vtt = ab(30720)
            s7 = af(32256)
            Sst = af(40448)
            Sbf = ab(42496)
            scT = ab(43520)
            osb = af(44032)
            junk = af(45056)
            onb = af(46080)
            ytm = ab(47104)
            stat = af(48128)
            qdc = ab(48192)
            qex = ab(48704)
            csb = ab(52800)
            usl = af(54848)
            kds = ab(63040)

            def tm_pair(l, col0, func, dst, dkey, chunks):
                def ev(t, b1, b2):
                    act(vtt(0, (1, TW)), PS[b1](0, (1, TW)), func, [("ps", b1)], [("A", "vtt", 0)])
                    act(vtt(TW, (1, TW)), PS[b2](0, (1, TW)), func, [("ps", b2)], [("A", "vtt", 1)])
                    for jj in range(3):
                        j = t * 3 + jj
                        if j not in chunks:
                            continue
                        hh = h7()
                        tr([(PS7L[hh](d2 * 128, (1, 128)), vtt(d2 * TW + jj * 128, (1, 128)), IDB(0, (1, 128)))
                            for d2 in range(2)], [("A", "vtt", 0), ("A", "vtt", 1), "idb"], [("ps7", hh)])
                        cp(dst(j * 256, (1, 256)), PS7L[hh](0, (1, 256)), [("ps7", hh)], [dkey])
                pair_mm(l, col0, col0 + 128, ev)

            def v_tile_pair(l, h, chunks):
                tm_pair(l, 4096 + h * 256, AF.Identity, vtm, ("A", "vtm"), chunks)

            def k_transposes(h, chunks, phase1):
                for j in chunks:
                    hh = h7()
                    tr([(PS7L[hh](d2 * 128, (1, 128)), kT(d2 * T + j * 128, (1, 128)), IDB(0, (1, 128)))
                        for d2 in range(2)], [("A", "kT"), "idb"], [("ps7", hh)])
                    if phase1:
                        sc = TB(TC_K1 + j * 4 + h, (1, 1))
                    elif j < 8:
                        sc = TB(TC_K2 + h, (1, 1))
                    else:
                        sc = TB(TC_KS + h, (1, 1))
                    act(kdt(j * 256, (1, 256)), PS7L[hh](0, (1, 256)), AF.Identity, [("ps7", hh), "tabs"],
                        [("A", "kdt")], scale=sc)

            for h in range(4):
                rot_pair(l, 3072 + h * 256, kT, ("A", "kT"), rt)
                v_tile_pair(l, h, range(8))
                k_transposes(h, range(8), True)
                b = nbank()
                mm([(PS[b](dc * 256, (1, 256)),
                     [(kdt(j * 256 + dc * 128, (1, 128)), vtm(j * 256, (1, 256))) for j in range(8)])
                    for dc in range(2)], [("A", "kdt"), ("A", "vtm")], [("ps", b)])
                cp(s7(h * 512, (1, 512)), PS[b](0, (1, 512)), [("ps", b)], [("A", "s7")])
            P.dma("pool", lambda e: e.dma_start(out=cc2_in[l].ap(), in_=s7(0, (1, 2048))), ("cc2i", l),
                  [("A", "s7")], [("cc2in", l)])
            if not NOCC:
                P.dma("pool", lambda e: e.collective_compute("AllGather", ALU.bypass, replica_groups=PAIRS,
                                                             ins=[cc2_in[l].ap().opt()], outs=[cc2_out[l].ap().opt()]),
                      ("cc2", l), [("cc2in", l)], [("cc2out", l)], inc=1)
            cc2_src = cc2_in if NOCC else cc2_out
            cc2_key = ("cc2in", l) if NOCC else ("cc2out", l)

            chk(8 + 20 * l)
            for h in range(4):
                rot_pair(l, 2048 + h * 256, qT, ("A", "qT"), rt)
                rot_pair(l, 3072 + h * 256, kT, ("A", "kT"), rt)
                v_tile_pair(l, h, range(9))
                k_transposes(h, range(9), False)
                tm_pair(l, 5120 + h * 256, AF.Silu, sgt, ("A", "sgt"), range(9))
                spdma(Sst(0, (1, 512)), cc2_src[l].ap()[0:128, h * 512:(h + 1) * 512], [cc2_key], [("A", "S")], sem="Sst")
                ts(Sst(0, (1, 512)), Sst(0, (1, 512)), isb, None, ALU.mult, None, [("A", "S"), "tabs"], [("A", "S")])
                act(Sbf(0, (1, 512)), Sst(0, (1, 512)), AF.Copy, [("A", "S")], [("A", "Sbf")])
                g128 = float(GAM[h] ** 128)
                g8 = float(GAM[h] ** 8)
                P.mark("L%dh%d_chunks" % (l, h))
                cb_ctr = [0]

                def cbank():
                    b = cb_ctr[0] % 6
                    cb_ctr[0] += 1
                    return b

                def scores(j):
                    b = cbank()
                    mm([(PS[b](0, (1, 128)), [(kT(dc * T + j * 128, (1, 128)), qT(dc * T + j * 128, (1, 128)))
                                               for dc in range(2)])], [("A", "kT"), ("A", "qT")], [("ps", b)])
                    return b

                def premask(j, b):
                    s_ = j % 2
                    mk = TB((TC_MS if j == 8 else TC_MP) + h * 128, (1, 128))
                    tt(scT(s_ * 128, (1, 128)), PS[b](0, (1, 128)), mk, ALU.mult, [("ps", b), "tabs"], [("A", "scT", s_)])
                    if j < 8:
                        tt(qdc(0, (128, 2), (1, 128)), qT(j * 128, (T, 2), (1, 128)),
                           TB(TC_QP + h * 128, (0, 2), (1, 128)), ALU.mult, [("A", "qT"), "tabs"], [("A", "qdc")])

                def gn1(j, bo):
                    act(osb(0, (1, 256)), PS[bo](0, (1, 256)), AF.Copy, [("ps", bo)], [("A", "osb")])
                    P.op("act", lambda e: e.activation(junk(0, (1, 256)), osb(0, (1, 256)), AF.Identity,
                                                       scale=-1.0 / 256.0, accum_out=stat(1, (1, 1))),
                         [("A", "osb")], [("A", "junk"), ("A", "stat")])
                    P.op("act", lambda e: e.activation(junk(0, (1, 256)), osb(0, (1, 256)), AF.Square,
                                                       bias=stat(1, (1, 1)), accum_out=stat(2, (1, 1))),
                         [("A", "osb"), ("A", "stat")], [("A", "junk"), ("A", "stat")])
                    act(stat(3, (1, 1)), stat(2, (1, 1)), AF.Sqrt, [("A", "stat")], [("A", "stat3")],
                        bias=float(EPS), scale=1.0 / 256.0)

                def gn2(j):
                    pass

                def gn3(j):
                    ys = j % 2
                    P.op("dve", lambda e: e.reciprocal(stat(4, (1, 1)), stat(3, (1, 1))), [("A", "stat3")], [("A", "stat")])
                    stt(onb(0, (1, 256)), osb(0, (1, 256)), stat(1, (1, 1)), sgt(j * 256, (1, 256)), ALU.add, ALU.mult,
                        [("A", "osb"), ("A", "stat"), ("A", "sgt")], [("A", "onb")])
                    ts(ytm(ys * 256, (1, 256)), onb(0, (1, 256)), stat(4, (1, 1)), None, ALU.mult, None,
                       [("A", "onb"), ("A", "stat")], [("A", "ytm", ys)])
                    hh = h7()
                    tr([(PS7L[hh](d2 * 128, (1, 128)), ytm(ys * 256 + d2 * 128, (1, 128)), IDB(0, (1, 128))) for d2 in range(2)],
                       [("A", "ytm", ys), "idb"], [("ps7", hh)])
                    act(mixc(j * 128, (T, 2), (1, 128)), PS7L[hh](0, (128, 2), (1, 128)), AF.Copy, [("ps7", hh)],
                        [("A", "mixc")])

                bs = scores(0)
                premask(0, bs)
                for j in range(9):
                    samp = (j == 8)
                    s_ = j % 2
                    bo = cbank()
                    if not samp:
                        mm([(PS[bo](0, (1, 256)),
                             [(scT(s_ * 128, (1, 128)), vtm(j * 256, (1, 256)))] +
                             [(qdc(dc * 128, (1, 128)), Sbf(dc * 256, (1, 256))) for dc in range(2)])],
                           [("A", "scT", s_), ("A", "vtm"), ("A", "qdc"), ("A", "Sbf")], [("ps", bo)])
                        if j >= 1:
                            gn3(j - 1)
                        gn1(j, bo)
                        bu = cbank()
                        mm([(PS[bu](dc * 256, (1, 256)), [(kdt(j * 256 + dc * 128, (1, 128)), vtm(j * 256, (1, 256)))])
                            for dc in range(2)], [("A", "kdt"), ("A", "vtm")], [("ps", bu)])
                        bs = scores(j + 1)
                        stt(Sst(0, (1, 512)), Sst(0, (1, 512)), g128, PS[bu](0, (1, 512)), ALU.mult, ALU.add,
                            [("A", "S"), ("ps", bu)], [("A", "S")])
                        if j < 7:
                            act(Sbf(0, (1, 512)), Sst(0, (1, 512)), AF.Copy, [("A", "S")], [("A", "Sbf")])
                        else:
                            spdma(oret_p.ap()[l, h].rearrange("(c p) e -> p c e", p=128), Sst(0, (256, 2), (1, 256)),
                                  [("A", "S")], [], sem="Sst")
                        premask(j + 1, bs)
                    else:
                        o0_, l0_, r0_ = PS[bo](0, (1, 256)), scT(s_ * 128, (1, 128)), vtm(j * 256, (1, 256))
                        P.op("pe", lambda e, o0_=o0_, l0_=l0_, r0_=r0_: e.matmul(o0_, l0_, r0_, start=True, stop=False),
                             [("A", "scT", s_), ("A", "vtm")], [("ps", bo)])
                        for dc in range(2):
                            P.op("dve", lambda e: e.memset(qex(0, (1, 2048)), 0.0), [("A", "qex")], [("A", "qex")])
                            tt(qex(0, (136, 16), (1, 8)), qT(dc * T + 1024, (8, 16), (1, 8)),
                               TB(TC_QS + h * 128, (8, 16), (1, 8)), ALU.mult, [("A", "qT"), "tabs"], [("A", "qex")])
                            for s in range(16):
                                cs_ = (dc * 16 + s) % 4
                                d_out = csb(cs_ * 256, (1, 256))
                                d_in = sret_d.ap()[l, s, h, dc * 128:(dc + 1) * 128, :]
                                P.dma("pool", lambda e, d_out=d_out, d_in=d_in: e.dma_start(out=d_out, in_=d_in),
                                      ("csb", cs_), [], [("A", "csb", cs_)])
                                o_, l_, r_ = PS[bo](0, (1, 256)), qex(s * 128, (1, 128)), csb(cs_ * 256, (1, 256))
                                last_ = (dc == 1 and s == 15)
                                P.op("pe", lambda e, o_=o_, l_=l_, r_=r_, last_=last_: e.matmul(o_, l_, r_, start=False, stop=last_),
                                     [("A", "qex"), ("A", "csb", cs_)], [("ps", bo)])
                        gn3(j - 1)
                        gn1(j, bo)
                gn3(8)
                P.mark("L%dh%d_supd" % (l, h))
                def uload(u):
                    s, dc = u // 2, u % 2
                    spdma(usl((u % 4) * 256, (1, 256)), sret_d.ap()[l, s, h, dc * 128:(dc + 1) * 128, :],
                          [], [("A", "uin", u % 4)], sem=("uin", u % 4))
                for u in range(4):
                    uload(u)
                for u in range(32):
                    s, dc = u // 2, u % 2
                    k_ = s % 2
                    if dc == 0:
                        ts(kds(k_ * 256, (1, 256)), kdt(8 * 256, (1, 256)), TB(TC_OH + s, (1, 1)), None, ALU.mult, None,
                           [("A", "kdt"), "tabs"], [("A", "kds", k_)])
                    bu = cbank()
                    mm([(PS[bu](0, (1, 256)), [(kds(k_ * 256 + dc * 128, (1, 128)), vtm(8 * 256, (1, 256)))])],
                       [("A", "kds", k_), ("A", "vtm")], [("ps", bu)])
                    stt(usl(1024 + (u % 4) * 256, (1, 256)), usl((u % 4) * 256, (1, 256)), g8, PS[bu](0, (1, 256)),
                        ALU.mult, ALU.add, [("A", "uin", u % 4), ("ps", bu)], [("A", "uout", u % 4)])
                    spdma(oret_s.ap()[l, s, h, dc * 128:(dc + 1) * 128, :], usl(1024 + (u % 4) * 256, (1, 256)),
                          [("A", "uout", u % 4)], [], sem=("uout", u % 4))
                    if u + 4 < 32:
                        uload(u + 4)
                P.mark("L%dh%d_wout" % (l, h))
                wout_group(l, 1024 + h * 256, 2, mixc, ("A", "mixc"), tt_outer=False)
                P.mark("L%dh%d_end" % (l, h))

        def ple(l):
            pT = ab(36352)
            pin = af(40960)
            plt = af(43008)
            for j in range(NCH):
                s_ = j % 2
                spdma(pin(s_ * 256, (1, 256)), p_d.ap()[l, j * 128:(j + 1) * 128, :], [], [("A", "pin", s_)],
                      sem=("pin", s_))
                b = mbank()
                tr([(PS[b](i * 128, (1, 128)), pin(s_ * 256 + i * 128, (1, 128)), IDF) for i in range(2)],
                   [("A", "pin", s_), "tabs"], [("ps", b)])
                cp(pT(j * 128, (T, 2), (1, 128)), PS[b](0, (128, 2), (1, 128)), [("ps", b)], [("A", "pT")])
            for dc in range(16):
                wg, kg = wload(wtile("ple_gate", l, 0, 16, dc * 128), 16)
                wp, kp = wload(wtile("ple_proj", l, 0, 2, dc * 128), 2)
                for t in range(NTT):
                    ba, bb = nbank(), nbank()
                    mm([(PS[ba](0, (1, TW)), [(wg(kc), xb_rhs(kc, t * TW, TW)) for kc in range(16)])],
                       [kg, KX(t)], [("ps", ba)])
                    mm([(PS[bb](0, (1, TW)), [(wp(kc), pT(kc * T + t * TW, (1, TW))) for kc in range(2)])],
                       [kp, ("A", "pT")], [("ps", bb)])
                    s_ = t % 2
                    act(plt(s_ * TW, (1, TW)), PS[ba](0, (1, TW)), AF.Sigmoid, [("ps", ba)], [("A", "plt", s_)])
                    tt(plt(s_ * TW, (1, TW)), plt(s_ * TW, (1, TW)), PS[bb](0, (1, TW)), ALU.mult,
                       [("A", "plt", s_), ("ps", bb)], [("A", "plt", s_)])
                    xr = XR(dc * T + t * TW, (1, TW))
                    tt(xr, xr, plt(s_ * TW, (1, TW)), ALU.add, [("A", "plt", s_), KR(t)], [KR(t)])

        try:
            for l in range(NL):
                chk(1 + 20 * l)
                ffn(l, "ffn1")
                chk(2 + 20 * l)
                layernorm(l, 0)
                chk(3 + 20 * l)
                mixing(l)
                chk(10 + 20 * l)
                layernorm(l, 1)
                chk(11 + 20 * l)
                ffn(l, "ffn2")
                chk(12 + 20 * l)
                ple(l)
                chk(13 + 20 * l)
                layernorm(l, 2, final=(l == NL - 1))
        except _Stop:
            pass

        P.mark("out")
        P.fence(fence_nop)
        for j in range(NCH):
            sl = j % 2
            yo = af(sl * 8192)
            ky = ("A", "yo", sl)
            for q in range(4 if not KSUB & 4 else 0):
                b = mbank()
                tr([(PS[b](i * 128, (1, 128)), XR((q * 4 + i) * T + j * 128, (1, 128)), IDF) for i in range(4)],
                   [KR(j // 3), "tabs"], [("ps", b)])
                cp(yo(q * 512, (1, 512)), PS[b](0, (1, 512)), [("ps", b)], [ky], eng="act" if q % 2 else "dve")
            spdma(y_d.ap()[j * 128:(j + 1) * 128, :], yo(0, (1, 2048)), [ky], [], sem=("yo", sl))

        P.mark("end")
        _NC_CACHE["marks"] = P.marks
        P.emit()
    _NC_CACHE["used"] = set(I.keys()) | set(W.keys())
    _NC_CACHE["outs"] = set(O.keys())
    return nc


def _tables(hf):
    tab = np.zeros((128, NTAB), np.float64)
    pos = np.concatenate([hf * 1024 + np.arange(1024), np.tile(16384 + np.arange(8), 16)]).astype(np.float32)
    inv = (np.float32(10000.0) ** (-(np.arange(128, dtype=np.float32)) / np.float32(128))).astype(np.float32)
    ang = (pos[None, :] * inv[:, None]).astype(np.float32)
    tab[:, TC_COS:TC_COS + T] = np.cos(ang.astype(np.float64))
    tab[:, TC_SIN:TC_SIN + T] = np.sin(ang.astype(np.float64))
    m = np.arange(128)[:, None]
    lq = np.arange(128)[None, :]
    for h in range(4):
        g = GAM[h]
        mp = np.where(lq >= m, 0.0625 * g ** np.maximum(lq - m, 0).astype(np.float64), 0.0)
        tab[:, TC_MP + h * 128:TC_MP + (h + 1) * 128] = mp
        same = (m // 8) == (lq // 8)
        dj = (lq % 8) - (m % 8)
        ms = np.where(same & (dj >= 0), 0.0625 * g ** np.maximum(dj, 0).astype(np.float64), 0.0)
        tab[:, TC_MS + h * 128:TC_MS + (h + 1) * 128] = ms
        tab[:, TC_QP + h * 128:TC_QP + (h + 1) * 128] = (g ** (np.arange(128) + 1.0))[None, :]
        tab[:, TC_QS + h * 128:TC_QS + (h + 1) * 128] = (g ** ((np.arange(128) % 8) + 1.0))[None, :]
        for c in range(8):
            tab[:, TC_K1 + c * 4 + h] = 0.0625 * g ** (1023.0 - 128 * c - np.arange(128))
        tab[:, TC_K2 + h] = 0.0625 * g ** (127.0 - np.arange(128))
        tab[:, TC_KS + h] = 0.0625 * g ** (7.0 - (np.arange(128) % 8))
    for s in range(16):
        tab[:, TC_OH + s] = ((np.arange(128) // 8) == s).astype(np.float64)
    tab[:, TC_ISB] = float(hf)
    for gi in range(4):
        w = 2 << gi
        p16 = hf * 1024 + np.arange(16)
        tab[:, TC_CORR + gi * 16:TC_CORR + (gi + 1) * 16] = (w / np.minimum(w, p16 + 1.0))[None, :]
    tab[:, TC_ID:TC_ID + 128] = np.eye(128)
    tab[:, TC_ONES:TC_ONES + 128] = 1.0 / 2048.0
    return tab.astype(np.float32)


def kernel(x_prompt, x_sample, p_prompt, p_sample, cache_conv, cache_pool, state_ret,
           ln1_g, ln1_b, ffn1_w_gate, ffn1_w_up, ffn1_w_down, w_in, conv_w, pool_w, pool_scale,
           w_out, ln2_g, ln2_b, ffn2_w_gate, ffn2_w_up, ffn2_w_down, ple_gate, ple_proj, ln3_g, ln3_b):
    f = lambda a: np.ascontiguousarray(np.asarray(a, dtype=np.float32))
    x_prompt, x_sample, p_prompt, p_sample = f(x_prompt), f(x_sample), f(p_prompt), f(p_sample)
    cache_conv, cache_pool, state_ret = f(cache_conv), f(cache_pool), f(state_ret)
    vecs = np.zeros((128, NV), np.float32)
    for l in range(NL):
        base = l * 112
        for k, a in enumerate((ln1_g, ln1_b, ln2_g, ln2_b, ln3_g, ln3_b)):
            vecs[:, base + k * 16:base + (k + 1) * 16] = f(a)[l].reshape(16, 128).T
        cw = f(conv_w)[l]
        for ci in range(4):
            for j in range(3):
                vecs[:, base + 96 + ci * 3 + j] = cw[j, ci * 128:(ci + 1) * 128]
        vecs[:, base + 108:base + 112] = f(pool_scale)[l].reshape(4, 128).T
    wts = {"ffn1_w_gate": f(ffn1_w_gate), "ffn1_w_up": f(ffn1_w_up), "ffn1_w_down": f(ffn1_w_down),
           "w_in": f(w_in), "pool_w": f(pool_w), "w_out": f(w_out),
           "ffn2_w_gate": f(ffn2_w_gate), "ffn2_w_up": f(ffn2_w_up), "ffn2_w_down": f(ffn2_w_down),
           "ple_gate": f(ple_gate), "ple_proj": f(ple_proj)}
    tabs = [_tables(0), _tables(1)]
    in_maps = []
    for c in range(8):
        pr, hf = c // 2, c % 2
        sl = slice(16 * c, 16 * c + 16)
        m = {
            "x": np.concatenate([x_prompt[pr, hf * 1024:(hf + 1) * 1024], x_sample[sl].reshape(128, D)], 0),
            "p": np.concatenate([p_prompt[:, pr, hf * 1024:(hf + 1) * 1024], p_sample[:, sl].reshape(NL, 128, 256)], 1),
            "cconv": cache_conv[:, sl].reshape(NL, 32, 512),
            "cpool": cache_pool[:, sl].reshape(NL, 240, 512),
            "sret": state_ret[:, sl],
            "vecs": vecs, "tabs": tabs[hf],
        }
        m = {k: np.ascontiguousarray(v) for k, v in m.items()}
        m.update(wts)
        in_maps.append(m)
    if os.environ.get("KDBG_MAPS"):
        return in_maps
    if "nc" not in _NC_CACHE:
        _NC_CACHE["nc"] = build_program()
    used = _NC_CACHE["used"]
    in_maps = [{k: v for k, v in m.items() if k in used} for m in in_maps]
    res = run_bass_kernel_spmd(_NC_CACHE["nc"], in_maps, core_ids=list(range(8)))
    R = res.results
    y_prompt = np.zeros((4, 2048, D), np.float32)
    y_sample = np.zeros((128, 8, D), np.float32)
    conv_p = np.zeros((NL, 4, 2, 512), np.float32)
    pool_p = np.zeros((NL, 4, 15, 512), np.float32)
    ret_p = np.zeros((NL, 4, 4, 256, 256), np.float32)
    conv_s = np.zeros((NL, 128, 2, 512), np.float32)
    pool_s = np.zeros((NL, 128, 15, 512), np.float32)
    ret_s = np.zeros((NL, 128, 4, 256, 256), np.float32)
    for c in range(8):
        pr, hf = c // 2, c % 2
        sl = slice(16 * c, 16 * c + 16)
        r = R[c]
        y = np.asarray(r["y"])
        y_prompt[pr, hf * 1024:(hf + 1) * 1024] = y[:1024]
        y_sample[sl] = y[1024:].reshape(16, 8, D)
        if "oconv_s" in r:
            conv_s[:, sl] = np.asarray(r["oconv_s"]).reshape(NL, 16, 2, 512)
        if "opool_s" in r:
            pool_s[:, sl] = np.asarray(r["opool_s"]).reshape(NL, 16, 15, 512)
        if "oret_s" in r:
            ret_s[:, sl] = np.asarray(r["oret_s"])
        if hf == 1:
            if "oconv_p" in r:
                conv_p[:, pr] = np.asarray(r["oconv_p"])
            if "opool_p" in r:
                pool_p[:, pr] = np.asarray(r["opool_p"])
            if "oret_p" in r:
                ret_p[:, pr] = np.asarray(r["oret_p"])
    return (y_prompt, y_sample, conv_p, pool_p, ret_p, conv_s, pool_s, ret_s)
```

```python
import math
import os
import contextlib
import numpy as np
import concourse.bass as bass
import concourse.mybir as mybir
from concourse.bass_utils import run_bass_kernel_spmd

F32 = mybir.dt.float32
BF16 = mybir.dt.bfloat16
AF = mybir.ActivationFunctionType
ALU = mybir.AluOpType

D = 2048
FF = 5632
NL = 2
T = 1152
TPR = 1024
NTT = 3
TW = 384
NCH = 9
ALPHA = 4 ** 0.25
EPS = 1e-5
GAM = [1.0 - 2.0 ** (-5 - h) for h in range(4)]
NSLOT = 4
ARENA = 64512
NV = 224
TC_COS, TC_SIN, TC_MP, TC_MS, TC_QP, TC_QS = 0, 1152, 2304, 2816, 3328, 3840
TC_K1, TC_K2, TC_KS, TC_OH, TC_ISB, TC_CORR, TC_ID, TC_ONES = 4352, 4384, 4388, 4392, 4408, 4416, 4480, 4608
NTAB = 4736


class _Op:
    __slots__ = ("eng", "fn", "deps", "is_dma", "dma_sem", "dma_val", "signal", "count", "idx", "inc", "near")


SMALL_KEYS = {"stat", "stat3", "xs1", "xr1", "cvt", "pvt", "mean", "rstd", "corr"}


class Prog:
    ENGS = ("pe", "act", "dve", "pool", "sp")

    def __init__(self, nc):
        self.nc = nc
        self.ops = []
        self.last_w = {}
        self.readers = {}
        self.dma_sems = {}
        self.fence_op = None
        self.pe_n = 0
        self.marks = []

    def _add(self, eng, fn, reads, writes, is_dma, dma_sem, inc=16):
        op = _Op()
        op.eng, op.fn, op.is_dma, op.dma_sem = eng, fn, is_dma, dma_sem
        op.signal = False
        op.count = 0
        op.inc = inc
        op.idx = len(self.ops)
        op.near = any(isinstance(k, tuple) and len(k) > 1 and k[0] == "A" and k[1] in SMALL_KEYS
                      for k in list(reads) + list(writes))
        deps = set()
        for k in list(reads) + list(writes):
            if (self.fence_op is not None and isinstance(k, tuple) and k and k[0] == "A"
                    and k not in self.last_w and k not in self.readers):
                deps.add(self.fence_op)
        for r in reads:
            w = self.last_w.get(r)
            if w is not None:
                deps.add(w)
        for w_ in writes:
            w = self.last_w.get(w_)
            if w is not None:
                deps.add(w)
            for rd in self.readers.get(w_, ()):
                deps.add(rd)
        for r in reads:
            if isinstance(r, tuple) and r and r[0] in ("ps", "ps7"):
                for rd in self.readers.get(r, ()):
                    deps.add(rd)
        op.deps = deps
        for r in reads:
            self.readers.setdefault(r, []).append(op.idx)
        for w_ in writes:
            self.last_w[w_] = op.idx
            self.readers[w_] = []
        if is_dma:
            ent = self.dma_sems.setdefault(dma_sem, [None, 0])
            ent[1] += inc
            op.dma_val = ent[1]
        self.ops.append(op)
        return op

    def op(self, eng, fn, reads=(), writes=(), n=1):
        if eng == "pe":
            self.pe_n += n
        return self._add(eng, fn, reads, writes, False, None)

    def mark(self, label):
        self.marks.append((label, self.pe_n))

    def dma(self, eng, fn, sem, reads=(), writes=(), inc=16):
        return self._add(eng, fn, reads, writes, True, sem, inc)

    def fence(self, fn):
        akeys = [k for k in set(list(self.last_w) + list(self.readers))
                 if isinstance(k, tuple) and k and k[0] == "A"]
        op = self._add("dve", fn, akeys, akeys, False, None)
        for k in akeys:
            self.last_w.pop(k, None)
            self.readers.pop(k, None)
        self.fence_op = op.idx

    def emit(self):
        nc = self.nc
        ops = self.ops
        epos = {e: 0 for e in self.ENGS}
        for op in ops:
            epos[op.eng] += 1
            op.count = epos[op.eng]
        pos = {op.idx: op.count for op in ops}
        self._pos = pos
        NEAR = 6

        def same_eng_skip(op, dop):
            if dop.eng != op.eng or op.is_dma:
                return False
            if (op.near or dop.near) and op.eng in ("act", "dve", "pool") and pos[op.idx] - pos[dop.idx] <= NEAR:
                return False
            return True
        self._skip = same_eng_skip
        for op in ops:
            for d in op.deps:
                dop = ops[d]
                if dop.is_dma:
                    continue
                if same_eng_skip(op, dop):
                    continue
                dop.signal = True
        cnt = {e: 0 for e in self.ENGS}
        for op in ops:
            if op.signal:
                cnt[op.eng] += 1
            op.count = cnt[op.eng]
        by_eng = {e: [o for o in ops if o.eng == e] for e in self.ENGS}
        with contextlib.ExitStack() as st:
            esem = {e: st.enter_context(nc.semaphore("sem_" + e)) for e in self.ENGS}
            for i, (name, ent) in enumerate(self.dma_sems.items()):
                ent[0] = st.enter_context(nc.semaphore("dsem_%d" % i))
            block = st.enter_context(nc.Block())
            dma_final = [(ent[0], ent[1]) for ent in self.dma_sems.values()]

            def run(ename, e):
                waited = {}
                for op in by_eng[ename]:
                    need = {}
                    for d in op.deps:
                        dop = ops[d]
                        if dop.is_dma:
                            key = ("d", dop.dma_sem)
                            sem, val = self.dma_sems[dop.dma_sem][0], dop.dma_val
                        else:
                            if self._skip(op, dop):
                                continue
                            key = ("e", dop.eng)
                            sem, val = esem[dop.eng], dop.count
                        if need.get(key, (None, -1))[1] < val:
                            need[key] = (sem, val)
                    for key, (sem, val) in need.items():
                        if waited.get(key, -1) >= val:
                            continue
                        e.wait_ge(sem, val)
                        waited[key] = val
                    ins = op.fn(e)
                    if op.is_dma:
                        ins.then_inc(self.dma_sems[op.dma_sem][0], op.inc)
                    elif op.signal:
                        ins.then_inc(esem[ename], 1)
                if ename == "sp":
                    for sem, val in dma_final:
                        e.wait_ge(sem, val)
                    for en in self.ENGS:
                        if en != "sp" and cnt[en] > 0:
                            e.wait_ge(esem[en], cnt[en])

            @block.tensor
            def _(e):
                run("pe", e)

            @block.scalar
            def _(e):
                run("act", e)

            @block.vector
            def _(e):
                run("dve", e)

            @block.gpsimd
            def _(e):
                run("pool", e)

            @block.sync
            def _(e):
                run("sp", e)


class Buf:
    def __init__(self, ap2d, off=0):
        self.t = ap2d.tensor
        self.base = ap2d.offset + off
        self.ps = ap2d.ap[0][0]

    def __call__(self, off, *dims, p0=0, pn=128):
        return bass.AP(self.t, self.base + p0 * self.ps + off, [[self.ps, pn]] + [list(d) for d in dims])


_NC_CACHE = {}


def build_program():
    nc = bass.Bass("TRN2", target_bir_lowering=False)
    dt_in = lambda n, s: nc.dram_tensor(n, s, F32, kind="ExternalInput")
    dt_out = lambda n, s: nc.dram_tensor(n, s, F32, kind="ExternalOutput")
    ISH = {"x": [T, D], "p": [NL, T, 256], "cconv": [NL, 32, 512], "cpool": [NL, 240, 512],
           "sret": [NL, 16, 4, 256, 256], "vecs": [128, NV], "tabs": [128, NTAB]}

    class _LazyI(dict):
        def __missing__(self, nm):
            self[nm] = dt_in(nm, ISH[nm])
            return self[nm]
    I = _LazyI()

    class _L:
        def __init__(self, nm):
            self.nm = nm

        def ap(self):
            return I[self.nm].ap()
    x_d, p_d, cconv_d, cpool_d, sret_d, vecs_d, tabs_d = (_L(n) for n in ("x", "p", "cconv", "cpool", "sret", "vecs", "tabs"))
    WSH = {"ffn1_w_gate": [NL, D, FF], "ffn1_w_up": [NL, D, FF], "ffn1_w_down": [NL, FF, D],
           "w_in": [NL, D, 6144], "pool_w": [NL, 4, 128, 128], "w_out": [NL, D, D],
           "ffn2_w_gate": [NL, D, FF], "ffn2_w_up": [NL, D, FF], "ffn2_w_down": [NL, FF, D],
           "ple_gate": [NL, D, D], "ple_proj": [NL, 256, D]}

    class _LazyW(dict):
        def __missing__(self, nm):
            self[nm] = dt_in(nm, WSH[nm])
            return self[nm]
    W = _LazyW()
    OSH = {"y": [T, D], "oconv_s": [NL, 32, 512], "opool_s": [NL, 240, 512], "oret_s": [NL, 16, 4, 256, 256],
           "oconv_p": [NL, 2, 512], "opool_p": [NL, 15, 512], "oret_p": [NL, 4, 256, 256]}

    class _LazyO(dict):
        def __missing__(self, nm):
            self[nm] = dt_out(nm, OSH[nm])
            return self[nm]
    O = _LazyO()

    class _LO:
        def __init__(self, nm):
            self.nm = nm

        def ap(self):
            return O[self.nm].ap()
    y_d, oconv_s, opool_s, oret_s, oconv_p, opool_p, oret_p = (
        _LO(n) for n in ("y", "oconv_s", "opool_s", "oret_s", "oconv_p", "opool_p", "oret_p"))

    class _LazyC(dict):
        def __missing__(self, key):
            nm, l = key
            shp = {"cc1_in": [128, 72], "cc1_out": [256, 72], "cc2_in": [128, 2048], "cc2_out": [256, 2048]}[nm]
            self[key] = nc.dram_tensor("%s%d" % (nm, l), shp, F32)
            return self[key]
    CC = _LazyC()

    class _LC:
        def __init__(self, nm):
            self.nm = nm

        def __getitem__(self, l):
            return CC[(self.nm, l)]
    cc1_in, cc1_out, cc2_in, cc2_out = _LC("cc1_in"), _LC("cc1_out"), _LC("cc2_in"), _LC("cc2_out")
    PAIRS = [[0, 1], [2, 3], [4, 5], [6, 7]]

    P = Prog(nc)
    stage = int(os.environ.get("KSTAGE", "999"))

    class _Stop(Exception):
        pass

    def chk(n):
        P.mark("stage%d" % n)
        if n > stage:
            raise _Stop()

    with contextlib.ExitStack() as st:
        sbt = lambda n, s, d: st.enter_context(nc.sbuf_tensor("sb_" + n, s, d))
        xres_t = sbt("xres", [128, 16 * T], F32)
        xbf_t = sbt("xbf", [128, 16 * T], BF16)
        tabs_t = sbt("tabs", [128, NTAB], F32)
        vecs_t = sbt("vecs", [128, NV], F32)
        valp_t = sbt("valp", [128, NV], F32)
        idb_t = sbt("idb", [128, 128], BF16)
        wsl_t = sbt("wsl", [128, NSLOT * 2048], BF16)
        ar_t = sbt("arena", [128, ARENA // 4], F32)
        psb = [st.enter_context(nc.psum_tensor("ps%d" % i, [128, 512], F32)) for i in range(6)]
        ps7_t = [st.enter_context(nc.psum_tensor("psb%d" % i, [128, 1024], BF16)) for i in range(2)]

        XR = Buf(xres_t[:, :])
        XB = Buf(xbf_t[:, :])
        TB = Buf(tabs_t[:, :])
        VC = Buf(vecs_t[:, :])
        VA = Buf(valp_t[:, :])
        IDB = Buf(idb_t[:, :])
        WS = Buf(wsl_t[:, :])
        AF32 = Buf(ar_t[:, :])
        ABF = Buf(ar_t[:, :].bitcast(BF16))
        PS = [Buf(b[:, :]) for b in psb]
        PS7L = [Buf(t_[:, :]) for t_ in ps7_t]

        def af(off_b):
            return Buf(ar_t[:, :], off_b // 4)

        def ab(off_b):
            return Buf(ar_t[:, :].bitcast(BF16), off_b // 2)

        IDF = TB(TC_ID, (1, 128))
        ONES = TB(TC_ONES, (1, 128))

        bank_ctr = [0]

        def nbank():
            b = bank_ctr[0] % 4
            bank_ctr[0] += 1
            return b

        misc_ctr = [0]

        def mbank():
            b = 4 + misc_ctr[0] % 2
            misc_ctr[0] += 1
            return b

        h7_ctr = [0]

        def h7():
            b = h7_ctr[0] % 2
            h7_ctr[0] += 1
            return b

        def mm(groups, reads, writes):
            def fn(e):
                ins = None
                for out, pairs in groups:
                    n = len(pairs)
                    for i, (l, r) in enumerate(pairs):
                        ins = e.matmul(out, l, r, start=(i == 0), stop=(i == n - 1))
                return ins
            P.op("pe", fn, reads, writes, n=sum(len(p_) for _, p_ in groups))

        def tr(items, reads, writes):
            def fn(e):
                ins = None
                for o, i_, idn in items:
                    ins = e.transpose(o, i_, idn)
                return ins
            P.op("pe", fn, reads, writes, n=len(items))

        def act(out, in_, func, reads, writes, bias=None, scale=None):
            kw = {}
            if bias is not None:
                kw["bias"] = bias
            if scale is not None:
                kw["scale"] = scale
            P.op("act", lambda e: e.activation(out, in_, func, **kw), reads, writes)

        def tt(out, in0, in1, op, reads, writes, eng="dve"):
            P.op(eng, lambda e: e.tensor_tensor(out, in0, in1, op), reads, writes)

        def ts(out, in0, s1, s2, op0, op1, reads, writes, eng="dve"):
            if s2 is None:
                P.op(eng, lambda e: e.tensor_scalar(out, in0, s1, None, op0), reads, writes)
            else:
                P.op(eng, lambda e: e.tensor_scalar(out, in0, s1, s2, op0, op1), reads, writes)

        def stt(out, in0, sc, in1, op0, op1, reads, writes, eng="dve"):
            P.op(eng, lambda e: e.scalar_tensor_tensor(out, in0, sc, in1, op0, op1), reads, writes)

        def cp(out, in_, reads, writes, eng="dve"):
            if eng == "act":
                P.op(eng, lambda e: e.activation(out, in_, AF.Copy), reads, writes)
            else:
                P.op(eng, lambda e: e.tensor_copy(out, in_), reads, writes)

        sp_ctr = [0]

        def spdma(out, in_, reads, writes, sem=None):
            if sem is None:
                sem = ("sp", sp_ctr[0] % 8)
                sp_ctr[0] += 1
            P.dma("sp", lambda e: e.dma_start(out=out, in_=in_), sem, reads, writes)

        ws_ctr = [0]

        def wload(dram_ap, nk):
            s = ws_ctr[0] % NSLOT
            ws_ctr[0] += 1
            key = ("w", s)
            out = WS(s * 2048, (128, nk), (1, 128))
            P.dma("pool", lambda e: e.dma_start(out=out, in_=dram_ap), ("w", s), (), [key])
            return (lambda kc, s=s: WS(s * 2048 + kc * 128, (1, 128))), key

        def wtile(name, l, r0, nk, c0):
            return W[name].ap()[l][r0:r0 + nk * 128, c0:c0 + 128].rearrange("(k p) c -> p k c", p=128)

        def xb_rhs(kc, t0, n):
            return XB(kc * T + t0, (1, n))

        KX = lambda t: ("xbf", t)
        KR = lambda t: ("xres", t)

        spdma(tabs_t[:, :], tabs_d.ap(), (), ["tabs"], sem="c0")
        spdma(vecs_t[:, :], vecs_d.ap(), (), ["vecs"], sem="c1")
        KSUB = int(os.environ.get("KSUB", "0"))
        if not KSUB & 1:
            ts(valp_t[:, :], vecs_t[:, :], float(ALPHA), None, ALU.mult, None, ["vecs"], ["valp"])
        if not KSUB & 8:
            cp(idb_t[:, :], IDF, ["tabs"], ["idb"])

        for j in range(NCH if not KSUB & 2 else 0):
            sl = j % 2
            xin = af(sl * 8192)
            kx = ("A", "xin", sl)
            spdma(xin(0, (1, 2048)), x_d.ap()[j * 128:(j + 1) * 128, :], (), [kx], sem=("xin", sl))
            for q in range(4):
                b = mbank()
                tr([(PS[b](i * 128, (1, 128)), xin((q * 4 + i) * 128, (1, 128)), IDF) for i in range(4)],
                   [kx, "tabs"], [("ps", b)])
                o_r = XR(q * 4 * T + j * 128, (T, 4), (1, 128))
                o_b = XB(q * 4 * T + j * 128, (T, 4), (1, 128))
                src = PS[b](0, (128, 4), (1, 128))
                if not KSUB & 16:
                    act(o_r, src, AF.Identity, [("ps", b)], [KR(j // 3)], scale=float(ALPHA))
                if not KSUB & 32:
                    cp(o_b, src, [("ps", b)], [KX(j // 3)])

        fence_nop = lambda e: e.tensor_copy(valp_t[:, 0:1], valp_t[:, 0:1])

        def ffn(l, pre):
            P.fence(lambda e: e.tensor_copy(valp_t[:, 0:1], valp_t[:, 0:1]))
            hb = ab(0)
            sg = af(25344)
            sgc = [0]
            for g in range(4):
                for fi in range(11):
                    ft = g * 11 + fi
                    wg, kg = wload(wtile(pre + "_w_gate", l, 0, 16, ft * 128), 16)
                    wu, ku = wload(wtile(pre + "_w_up", l, 0, 16, ft * 128), 16)
                    for t in range(NTT):
                        ba, bb = nbank(), nbank()
                        mm([(PS[ba](0, (1, TW)), [(wg(kc), xb_rhs(kc, t * TW, TW)) for kc in range(16)])],
                           [kg, KX(t)], [("ps", ba)])
                        mm([(PS[bb](0, (1, TW)), [(wu(kc), xb_rhs(kc, t * TW, TW)) for kc in range(16)])],
                           [ku, KX(t)], [("ps", bb)])
                        s_ = sgc[0] % 3
                        sgc[0] += 1
                        ksg = ("A", "sg", s_)
                        act(sg(s_ * TW, (1, TW)), PS[ba](0, (1, TW)), AF.Silu, [("ps", ba)], [ksg])
                        tt(hb(fi * T + t * TW, (1, TW)), sg(s_ * TW, (1, TW)), PS[bb](0, (1, TW)), ALU.mult,
                           [ksg, ("ps", bb)], [("A", "h", t)])
                def down(dc, t, wd, kd):
                    b = nbank()
                    mm([(PS[b](0, (1, TW)), [(wd(fi), hb(fi * T + t * TW, (1, TW))) for fi in range(11)])],
                       [kd, ("A", "h", t)], [("ps", b)])
                    xr = XR(dc * T + t * TW, (1, TW))
                    stt(xr, PS[b](0, (1, TW)), 0.5, xr, ALU.mult, ALU.add, [("ps", b), KR(t)], [KR(t)])
                if True:
                    for dc in range(16):
                        wd, kd = wload(wtile(pre + "_w_down", l, g * 1408, 11, dc * 128), 11)
                        for t in range(NTT):
                            down(dc, t, wd, kd)
                else:
                    for t in range(NTT):
                        for dc in range(16):
                            wd, kd = wload(wtile(pre + "_w_down", l, g * 1408, 11, dc * 128), 11)
                            down(dc, t, wd, kd)

        def layernorm(l, which, final=False):
            gcol = l * 112 + which * 32
            bcol = gcol + 16
            P.fence(fence_nop)
            mean = af(30208)
            rstd = af(30208 + 1536)
            sq = af(30208 + 3072)
            for t in range(NTT):
                kt = KR(t)
                b = mbank()
                mm([(PS[b](0, (1, TW)), [(ONES, XR(dc * T + t * TW, (1, TW))) for dc in range(16)])],
                   [kt, "tabs"], [("ps", b)])
                cp(mean(0, (1, TW)), PS[b](0, (1, TW)), [("ps", b)], [("A", "mean")])
                xr3 = XR(t * TW, (T, 16), (1, TW))
                tt(xr3, xr3, mean(0, (0, 16), (1, TW)), ALU.subtract, [kt, ("A", "mean")], [kt])
                b2 = mbank()
                for dc in range(16):
                    s_ = dc % 2
                    act(sq(s_ * TW, (1, TW)), XR(dc * T + t * TW, (1, TW)), AF.Square, [kt], [("A", "sq", s_)])
                    o = PS[b2](0, (1, TW))
                    l_, r_ = ONES, sq(s_ * TW, (1, TW))
                    P.op("pe", (lambda e, o=o, l_=l_, r_=r_, dc=dc: e.matmul(o, l_, r_, start=(dc == 0), stop=(dc == 15))),
                         [("A", "sq", s_), "tabs"], [("ps", b2)])
                act(rstd(0, (1, TW)), PS[b2](0, (1, TW)), AF.Sqrt, [("ps", b2)], [("A", "rstd")], bias=float(EPS))
                P.op("dve", lambda e: e.reciprocal(rstd(0, (1, TW)), rstd(0, (1, TW))), [("A", "rstd")], [("A", "rstd")])
                tt(xr3, xr3, rstd(0, (0, 16), (1, TW)), ALU.mult, [kt, ("A", "rstd")], [kt])
                for dc in range(16):
                    xr = XR(dc * T + t * TW, (1, TW))
                    if not final:
                        act(XB(dc * T + t * TW, (1, TW)), xr, AF.Identity, [kt, "vecs"], [KX(t)],
                            bias=VC(bcol + dc, (1, 1)), scale=VC(gcol + dc, (1, 1)))
                        act(xr, xr, AF.Identity, [kt, "valp"], [kt],
                            bias=VA(bcol + dc, (1, 1)), scale=VA(gcol + dc, (1, 1)))
                    else:
                        act(xr, xr, AF.Identity, [kt, "vecs"], [kt],
                            bias=VC(bcol + dc, (1, 1)), scale=VC(gcol + dc, (1, 1)))

        def wout_group(l, r0, nk, mixbuf, mixkey, tt_outer=False):
            def one(dc, t, w, kw):
                b = nbank()
                mm([(PS[b](0, (1, TW)), [(w(kc), mixbuf(kc * T + t * TW, (1, TW))) for kc in range(nk)])],
                   [kw, mixkey], [("ps", b)])
                xr = XR(dc * T + t * TW, (1, TW))
                tt(xr, PS[b](0, (1, TW)), xr, ALU.add, [("ps", b), KR(t)], [KR(t)])
            if not tt_outer:
                for dc in range(16):
                    w, kw = wload(wtile("w_out", l, r0, nk, dc * 128), nk)
                    for t in range(NTT):
                        one(dc, t, w, kw)
            else:
                for t in range(NTT):
                    for dc in range(16):
                        w, kw = wload(wtile("w_out", l, r0, nk, dc * 128), nk)
                        one(dc, t, w, kw)

        def zmm(l, col0, evac):
            w, kw = wload(wtile("w_in", l, 0, 16, col0), 16)
            for t in range(NTT):
                b = nbank()
                mm([(PS[b](0, (1, TW)), [(w(kc), xb_rhs(kc, t * TW, TW)) for kc in range(16)])],
                   [kw, KX(t)], [("ps", b)])
                evac(t, b)

        def pair_mm(l, colA, colB, evac):
            w1, k1 = wload(wtile("w_in", l, 0, 16, colA), 16)
            w2, k2 = wload(wtile("w_in", l, 0, 16, colB), 16)
            for t in range(NTT):
                b1, b2 = nbank(), nbank()
                mm([(PS[b1](0, (1, TW)), [(w1(kc), xb_rhs(kc, t * TW, TW)) for kc in range(16)])],
                   [k1, KX(t)], [("ps", b1)])
                mm([(PS[b2](0, (1, TW)), [(w2(kc), xb_rhs(kc, t * TW, TW)) for kc in range(16)])],
                   [k2, KX(t)], [("ps", b2)])
                evac(t, b1, b2)

        def rot_pair(l, col0, outbuf, outkey, rt):
            def ev(t, b1, b2):
                cs = TB(TC_COS + t * TW, (1, TW))
                sn = TB(TC_SIN + t * TW, (1, TW))
                p1, p2 = PS[b1](0, (1, TW)), PS[b2](0, (1, TW))
                t1, t2 = rt(0, (1, TW)), rt(TW, (1, TW))
                ka, kb = ("A", "rt", 0), ("A", "rt", 1)
                tt(t1, p1, cs, ALU.mult, [("ps", b1), "tabs"], [ka])
                tt(t2, p2, sn, ALU.mult, [("ps", b2), "tabs"], [kb])
                tt(outbuf(t * TW, (1, TW)), t1, t2, ALU.subtract, [ka, kb], [outkey])
                tt(t1, p1, sn, ALU.mult, [("ps", b1), "tabs"], [ka])
                tt(t2, p2, cs, ALU.mult, [("ps", b2), "tabs"], [kb])
                tt(outbuf(T + t * TW, (1, TW)), t1, t2, ALU.add, [ka, kb], [outkey])
            pair_mm(l, col0, col0 + 128, ev)

        def mixing(l):
            P.fence(fence_nop)
            UW, PW = 1186, 1407
            uf = af(0)
            zp = af(18976)
            zc = af(41488)
            mixb = ab(46096)
            cst = af(55312)
            xs1 = af(59408)
            KU = lambda ci: ("A", "uf", ci)
            KZ = lambda gi: ("A", "zp", gi)
            vb = l * 112

            spdma(cst(0, (1, 512), pn=32), cconv_d.ap()[l], (), [("A", "cst")], sem="cst")
            for ci in range(4):
                b = mbank()
                tr([(PS[b](0, (1, 32)), cst(ci * 128, (1, 128), pn=32), TB(TC_ID, (1, 32), pn=32))],
                   [("A", "cst"), "tabs"], [("ps", b)])
                cp(uf(ci * UW + 1026, (10, 16), (1, 2)), PS[b](0, (2, 16), (1, 2)), [("ps", b)], [KU(ci)])
            for half, (r0, nr) in enumerate(((0, 128), (128, 112))):
                spdma(cst(0, (1, 512), pn=nr), cpool_d.ap()[l][r0:r0 + nr, :], (), [("A", "cst")], sem="cst")
                for gi in range(4):
                    b = mbank()
                    tr([(PS[b](0, (1, nr)), cst(gi * 128, (1, 128), pn=nr), TB(TC_ID, (1, nr), pn=nr))],
                       [("A", "cst"), "tabs"], [("ps", b)])
                    if half == 0:
                        cp(zp(gi * PW + 1039, (23, 8), (1, 15)), PS[b](0, (15, 8), (1, 15)), [("ps", b)], [KZ(gi)])
                        cp(zp(gi * PW + 1039 + 8 * 23, (1, 8)), PS[b](120, (1, 8)), [("ps", b)], [KZ(gi)])
                    else:
                        cp(zp(gi * PW + 1039 + 8 * 23 + 8, (1, 7)), PS[b](0, (1, 7)), [("ps", b)], [KZ(gi)])
                        cp(zp(gi * PW + 1039 + 9 * 23, (23, 7), (1, 15)), PS[b](7, (15, 7), (1, 15)),
                           [("ps", b)], [KZ(gi)])

            for ci in range(4):
                def ev_c(t, b):
                    act(zc(t * TW, (1, TW)), PS[b](0, (1, TW)), AF.Copy, [("ps", b)], [("A", "zc")])
                zmm(l, 512 + ci * 128, ev_c)

                def ev_h(t, b, ci=ci):
                    if t < 2:
                        tt(uf(ci * UW + 2 + t * TW, (1, TW)), PS[b](0, (1, TW)), zc(t * TW, (1, TW)), ALU.mult,
                           [("ps", b), ("A", "zc")], [KU(ci)])
                    else:
                        tt(uf(ci * UW + 2 + 768, (1, 256)), PS[b](0, (1, 256)), zc(768, (1, 256)), ALU.mult,
                           [("ps", b), ("A", "zc")], [KU(ci)])
                        tt(uf(ci * UW + 1028, (10, 16), (1, 8)), PS[b](256, (8, 16), (1, 8)),
                           zc(1024, (8, 16), (1, 8)), ALU.mult, [("ps", b), ("A", "zc")], [KU(ci)])
                zmm(l, 1024 + ci * 128, ev_h)
            for gi in range(4):
                def ev_p(t, b, gi=gi):
                    if t < 2:
                        act(zp(gi * PW + 15 + t * TW, (1, TW)), PS[b](0, (1, TW)), AF.Copy, [("ps", b)], [KZ(gi)])
                    else:
                        act(zp(gi * PW + 15 + 768, (1, 256)), PS[b](0, (1, 256)), AF.Copy, [("ps", b)], [KZ(gi)])
                        act(zp(gi * PW + 1039 + 15, (23, 16), (1, 8)), PS[b](256, (8, 16), (1, 8)), AF.Copy,
                            [("ps", b)], [KZ(gi)])
                zmm(l, 1536 + gi * 128, ev_p)

            chk(4 + 20 * l)
            allu = [KU(i) for i in range(4)]
            allz = [KZ(i) for i in range(4)]
            cp(xs1(0, (2, 4), (1, 2)), uf(1024, (UW, 4), (1, 2)), allu, [("A", "xs1")])
            cp(xs1(8, (15, 4), (1, 15)), zp(1024, (PW, 4), (1, 15)), allz, [("A", "xs1")])
            P.op("dve", lambda e: e.memset(xs1(68, (1, 4)), 0.0), [], [("A", "xs1")])
            P.dma("pool", lambda e: e.dma_start(out=cc1_in[l].ap(), in_=xs1(0, (1, 72))), ("cc1i", l),
                  [("A", "xs1")], [("cc1in", l)])
            NOCC = bool(int(os.environ.get("KSUB", "0")) & 64)
            if not NOCC:
                P.dma("pool", lambda e: e.collective_compute("AllGather", ALU.bypass, replica_groups=PAIRS,
                                                             ins=[cc1_in[l].ap().opt()], outs=[cc1_out[l].ap().opt()]),
                      ("cc1", l), [("cc1in", l)], [("cc1out", l)], inc=1)
                P.dma("pool", lambda e: e.dma_start(out=xs1(72, (1, 72)), in_=cc1_out[l].ap()[0:128, :]), ("cc1o", l),
                      [("cc1out", l)], [("A", "xr1")])
            else:
                P.dma("pool", lambda e: e.dma_start(out=xs1(72, (1, 72)), in_=cc1_in[l].ap()), ("cc1o", l),
                      [("cc1in", l)], [("A", "xr1")])
            isb = TB(TC_ISB, (1, 1))
            ts(uf(0, (UW, 4), (1, 2)), xs1(72, (2, 4), (1, 2)), isb, None, ALU.mult, None,
               [("A", "xr1"), "tabs"], allu)
            ts(zp(0, (PW, 4), (1, 15)), xs1(80, (15, 4), (1, 15)), isb, None, ALU.mult, None,
               [("A", "xr1"), "tabs"], allz)

            chk(5 + 20 * l)
            for ci in range(4):
                wc = lambda j, ci=ci: VC(vb + 96 + ci * 3 + j, (1, 1))
                kk = [KU(ci), "vecs"]
                ts(zc(0, (1, 1024)), uf(ci * UW, (1, 1024)), wc(0), None, ALU.mult, None, kk, [("A", "zc")])
                ts(zc(1024, (8, 16), (1, 8)), uf(ci * UW + 1026, (10, 16), (1, 8)), wc(0), None, ALU.mult, None,
                   kk, [("A", "zc")])
                for j in (1, 2):
                    stt(zc(0, (1, 1024)), uf(ci * UW + j, (1, 1024)), wc(j), zc(0, (1, 1024)), ALU.mult, ALU.add,
                        kk + [("A", "zc")], [("A", "zc")])
                    stt(zc(1024, (8, 16), (1, 8)), uf(ci * UW + 1026 + j, (10, 16), (1, 8)), wc(j),
                        zc(1024, (8, 16), (1, 8)), ALU.mult, ALU.add, kk + [("A", "zc")], [("A", "zc")])

                def ev_b(t, b, ci=ci):
                    tt(mixb(ci * T + t * TW, (1, TW)), PS[b](0, (1, TW)), zc(t * TW, (1, TW)), ALU.mult,
                       [("ps", b), ("A", "zc")], [("A", "mix")])
                zmm(l, ci * 128, ev_b)
            cvt = af(59408 + 576)
            cp(cvt(0, (34, 4), (2, 16), (1, 2)), uf(1026 + 8, (UW, 4), (10, 16), (1, 2)), allu, [("A", "cvt")])
            cp(cvt(32, (34, 4), (1, 2)), uf(1024, (UW, 4), (1, 2)), allu, [("A", "cvt")])
            b = mbank()
            tr([(PS[b](ci * 128, (1, 128), pn=34), cvt(ci * 34, (1, 34)), IDF) for ci in range(4)],
               [("A", "cvt"), "tabs"], [("ps", b)])
            cp(cst(0, (1, 512), pn=34), PS[b](0, (1, 512), pn=34), [("ps", b)], [("A", "cst")])
            spdma(oconv_s.ap()[l], cst(0, (1, 512), pn=32), [("A", "cst")], [], sem="cst")
            spdma(oconv_p.ap()[l], cst(0, (1, 512), p0=32, pn=2), [("A", "cst")], [], sem="cst")
            wout_group(l, 0, 4, mixb, ("A", "mix"))

            chk(6 + 20 * l)
            pa = af(0)
            pb_ = af(5632)
            dT = ab(11264)
            P.fence(fence_nop)
            wpl, kpl = wload(W["pool_w"].ap()[l].rearrange("g c d -> c g d"), 4)
            pvt = af(13568)
            for gi in range(4):
                wdw = 2 << gi
                src = lambda off, n, gi=gi: zp(gi * PW + off, (1, n))
                src3 = lambda off, n, gi=gi: zp(gi * PW + 1039 + off, (23, 16), (1, n))
                cur, cur3 = src, src3
                bufs = [pa, pb_]
                sh = 1
                step = 0
                lo = 0
                while sh < wdw:
                    ob = bufs[step % 2]
                    kin = [KZ(gi), ("A", "pp", 0), ("A", "pp", 1)]
                    lo2 = lo + sh
                    tt(ob(lo2, (1, 1039 - lo2)), cur(lo2, 1039 - lo2), cur(lo2 - sh, 1039 - lo2), ALU.add,
                       kin, [("A", "pp", step % 2)])
                    tt(ob(1039 + lo2, (23, 16), (1, 23 - lo2)), cur3(lo2, 23 - lo2), cur3(lo2 - sh, 23 - lo2), ALU.add,
                       kin, [("A", "pp", step % 2)])
                    cur = lambda off, n, ob=ob: ob(off, (1, n))
                    cur3 = lambda off, n, ob=ob: ob(1039 + off, (23, 16), (1, n))
                    lo = lo2
                    sh *= 2
                    step += 1
                kin = [KZ(gi), ("A", "pp", 0), ("A", "pp", 1), "tabs"]
                last = ("A", "pp", (step - 1) % 2)
                tt(cur(15, 16), cur(15, 16), TB(TC_CORR + gi * 16, (1, 16)), ALU.mult, kin, [last, ("A", "corr")])
                stt(dT(0, (1, 1024)), cur(15, 1024), 1.0 / wdw, src(15, 1024), ALU.mult, ALU.subtract,
                    kin, [("A", "dT")])
                stt(dT(1024, (8, 16), (1, 8)), cur3(15, 8), 1.0 / wdw, src3(15, 8), ALU.mult, ALU.subtract,
                    kin, [("A", "dT")])
                for t in range(NTT):
                    b = nbank()
                    mm([(PS[b](0, (1, TW)), [(wpl(gi), dT(t * TW, (1, TW)))])], [kpl, ("A", "dT")], [("ps", b)])
                    act(mixb(gi * T + t * TW, (1, TW)), PS[b](0, (1, TW)), AF.Identity, [("ps", b), "vecs"],
                        [("A", "mix")], scale=VC(vb + 108 + gi, (1, 1)))
                cp(pvt(gi * 255, (15, 16), (1, 15)), zp(gi * PW + 1039 + 8, (23, 16), (1, 15)), [KZ(gi)], [("A", "pvt")])
                cp(pvt(gi * 255 + 240, (1, 15)), zp(gi * PW + 1024, (1, 15)), [KZ(gi)], [("A", "pvt")])
            pst = af(55312)
            for blk, (c0, nr) in enumerate(((0, 128), (128, 127))):
                b = mbank()
                tr([(PS[b](gi * 128, (1, 128), pn=nr), pvt(gi * 255 + c0, (1, nr)), IDF) for gi in range(4)],
                   [("A", "pvt"), "tabs"], [("ps", b)])
                cp(pst(blk * 512, (1, 512), pn=nr), PS[b](0, (1, 512), pn=nr), [("ps", b)], [("A", "cst")])
            spdma(opool_s.ap()[l][0:128, :], pst(0, (1, 512)), [("A", "cst")], [], sem="cst")
            spdma(opool_s.ap()[l][128:240, :], pst(512, (1, 512), pn=112), [("A", "cst")], [], sem="cst")
            spdma(opool_p.ap()[l], pst(512, (1, 512), p0=112, pn=15), [("A", "cst")], [], sem="cst")
            wout_group(l, 512, 4, mixb, ("A", "mix"))

            chk(7 + 20 * l)
            P.fence(fence_nop)
            kT = ab(0)
            qT = ab(4608)
            kdt = ab(9216)
            vtm = ab(13824)
            sgt = ab(18432)
            mixc = ab(23040)
            rt = af(27648)
            vtt = ab(30720)
            s7 = af(32256)
            Sst = af(40448)
            Sbf = ab(42496)
            scT = ab(43520)
            osb = af(44032)
            junk = af(45056)
            onb = af(46080)
            ytm = ab(47104)
            stat = af(48128)
            qdc = ab(48192)
            qex = ab(48704)
            csb = ab(52800)
            usl = af(56896)
            kds = ab(63040)

            def tm_pair(l, col0, func, dst, dkey, chunks):
                def ev(t, b1, b2):
                    act(vtt(0, (1, TW)), PS[b1](0, (1, TW)), func, [("ps", b1)], [("A", "vtt", 0)])
                    act(vtt(TW, (1, TW)), PS[b2](0, (1, TW)), func, [("ps", b2)], [("A", "vtt", 1)])
                    for jj in range(3):
                        j = t * 3 + jj
                        if j not in chunks:
                            continue
                        hh = h7()
                        tr([(PS7L[hh](d2 * 128, (1, 128)), vtt(d2 * TW + jj * 128, (1, 128)), IDB(0, (1, 128)))
                            for d2 in range(2)], [("A", "vtt", 0), ("A", "vtt", 1), "idb"], [("ps7", hh)])
                        cp(dst(j * 256, (1, 256)), PS7L[hh](0, (1, 256)), [("ps7", hh)], [dkey])
                pair_mm(l, col0, col0 + 128, ev)

            def v_tile_pair(l, h, chunks):
                tm_pair(l, 4096 + h * 256, AF.Identity, vtm, ("A", "vtm"), chunks)

            def k_transposes(h, chunks, phase1):
                for j in chunks:
                    hh = h7()
                    tr([(PS7L[hh](d2 * 128, (1, 128)), kT(d2 * T + j * 128, (1, 128)), IDB(0, (1, 128)))
                        for d2 in range(2)], [("A", "kT"), "idb"], [("ps7", hh)])
                    if phase1:
                        sc = TB(TC_K1 + j * 4 + h, (1, 1))
                    elif j < 8:
                        sc = TB(TC_K2 + h, (1, 1))
                    else:
                        sc = TB(TC_KS + h, (1, 1))
                    act(kdt(j * 256, (1, 256)), PS7L[hh](0, (1, 256)), AF.Identity, [("ps7", hh), "tabs"],
                        [("A", "kdt")], scale=sc)

            for h in range(4):
                rot_pair(l, 3072 + h * 256, kT, ("A", "kT"), rt)
                v_tile_pair(l, h, range(8))
                k_transposes(h, range(8), True)
                b = nbank()
                mm([(PS[b](dc * 256, (1, 256)),
                     [(kdt(j * 256 + dc * 128, (1, 128)), vtm(j * 256, (1, 256))) for j in range(8)])
                    for dc in range(2)], [("A", "kdt"), ("A", "vtm")], [("ps", b)])
                cp(s7(h * 512, (1, 512)), PS[b](0, (1, 512)), [("ps", b)], [("A", "s7")])
            P.dma("pool", lambda e: e.dma_start(out=cc2_in[l].ap(), in_=s7(0, (1, 2048))), ("cc2i", l),
                  [("A", "s7")], [("cc2in", l)])
            if not NOCC:
                P.dma("pool", lambda e: e.collective_compute("AllGather", ALU.bypass, replica_groups=PAIRS,
                                                             ins=[cc2_in[l].ap().opt()], outs=[cc2_out[l].ap().opt()]),
                      ("cc2", l), [("cc2in", l)], [("cc2out", l)], inc=1)
            cc2_src = cc2_in if NOCC else cc2_out
            cc2_key = ("cc2in", l) if NOCC else ("cc2out", l)

            chk(8 + 20 * l)
            P.op("dve", lambda e: e.memset(qex(0, (1, 2048)), 0.0), [("A", "qex")], [("A", "qex")])
            for h in range(4):
                rot_pair(l, 2048 + h * 256, qT, ("A", "qT"), rt)
                rot_pair(l, 3072 + h * 256, kT, ("A", "kT"), rt)
                v_tile_pair(l, h, range(9))
                k_transposes(h, range(9), False)
                tm_pair(l, 5120 + h * 256, AF.Silu, sgt, ("A", "sgt"), range(9))
                spdma(Sst(0, (1, 512)), cc2_src[l].ap()[0:128, h * 512:(h + 1) * 512], [cc2_key], [("A", "S")], sem="Sst")
                ts(Sst(0, (1, 512)), Sst(0, (1, 512)), isb, None, ALU.mult, None, [("A", "S"), "tabs"], [("A", "S")])
                act(Sbf(0, (1, 512)), Sst(0, (1, 512)), AF.Copy, [("A", "S")], [("A", "Sbf")])
                g128 = float(GAM[h] ** 128)
                g8 = float(GAM[h] ** 8)
                P.mark("L%dh%d_chunks" % (l, h))
                cb_ctr = [0]

                def cbank():
                    b = cb_ctr[0] % 6
                    cb_ctr[0] += 1
                    return b

                def scores(j):
                    b = cbank()
                    mm([(PS[b](0, (1, 128)), [(kT(dc * T + j * 128, (1, 128)), qT(dc * T + j * 128, (1, 128)))
                                               for dc in range(2)])], [("A", "kT"), ("A", "qT")], [("ps", b)])
                    return b

                def premask(j, b):
                    s_ = j % 2
                    mk = TB((TC_MS if j == 8 else TC_MP) + h * 128, (1, 128))
                    tt(scT(s_ * 128, (1, 128)), PS[b](0, (1, 128)), mk, ALU.mult, [("ps", b), "tabs"], [("A", "scT", s_)])
                    if j < 8:
                        tt(qdc(0, (128, 2), (1, 128)), qT(j * 128, (T, 2), (1, 128)),
                           TB(TC_QP + h * 128, (0, 2), (1, 128)), ALU.mult, [("A", "qT"), "tabs"], [("A", "qdc")])

                def gn1(j, bo):
                    act(osb(0, (1, 256)), PS[bo](0, (1, 256)), AF.Copy, [("ps", bo)], [("A", "osb")])
                    P.op("act", lambda e: e.activation(junk(0, (1, 256)), osb(0, (1, 256)), AF.Identity,
                                                       scale=-1.0 / 256.0, accum_out=stat(1, (1, 1))),
                         [("A", "osb")], [("A", "junk"), ("A", "stat")])
                    P.op("act", lambda e: e.activation(junk(0, (1, 256)), osb(0, (1, 256)), AF.Square,
                                                       bias=stat(1, (1, 1)), accum_out=stat(2, (1, 1))),
                         [("A", "osb"), ("A", "stat")], [("A", "junk"), ("A", "stat")])
                    act(stat(3, (1, 1)), stat(2, (1, 1)), AF.Sqrt, [("A", "stat")], [("A", "stat3")],
                        bias=float(EPS), scale=1.0 / 256.0)

                def gn2(j):
                    pass

                def gn3(j):
                    ys = j % 2
                    P.op("dve", lambda e: e.reciprocal(stat(4, (1, 1)), stat(3, (1, 1))), [("A", "stat3")], [("A", "stat")])
                    stt(onb(0, (1, 256)), osb(0, (1, 256)), stat(1, (1, 1)), sgt(j * 256, (1, 256)), ALU.add, ALU.mult,
                        [("A", "osb"), ("A", "stat"), ("A", "sgt")], [("A", "onb")])
                    ts(ytm(ys * 256, (1, 256)), onb(0, (1, 256)), stat(4, (1, 1)), None, ALU.mult, None,
                       [("A", "onb"), ("A", "stat")], [("A", "ytm", ys)])
                    hh = h7()
                    tr([(PS7L[hh](d2 * 128, (1, 128)), ytm(ys * 256 + d2 * 128, (1, 128)), IDB(0, (1, 128))) for d2 in range(2)],
                       [("A", "ytm", ys), "idb"], [("ps7", hh)])
                    act(mixc(j * 128, (T, 2), (1, 128)), PS7L[hh](0, (128, 2), (1, 128)), AF.Copy, [("ps7", hh)],
                        [("A", "mixc")])

                bs = scores(0)
                premask(0, bs)
                for j in range(9):
                    samp = (j == 8)
                    s_ = j % 2
                    bo = cbank()
                    if not samp:
                        mm([(PS[bo](0, (1, 256)),
                             [(scT(s_ * 128, (1, 128)), vtm(j * 256, (1, 256)))] +
                             [(qdc(dc * 128, (1, 128)), Sbf(dc * 256, (1, 256))) for dc in range(2)])],
                           [("A", "scT", s_), ("A", "vtm"), ("A", "qdc"), ("A", "Sbf")], [("ps", bo)])
                        if j >= 1:
                            gn3(j - 1)
                        gn1(j, bo)
                        bu = cbank()
                        mm([(PS[bu](dc * 256, (1, 256)), [(kdt(j * 256 + dc * 128, (1, 128)), vtm(j * 256, (1, 256)))])
                            for dc in range(2)], [("A", "kdt"), ("A", "vtm")], [("ps", bu)])
                        bs = scores(j + 1)
                        stt(Sst(0, (1, 512)), Sst(0, (1, 512)), g128, PS[bu](0, (1, 512)), ALU.mult, ALU.add,
                            [("A", "S"), ("ps", bu)], [("A", "S")])
                        if j < 7:
                            act(Sbf(0, (1, 512)), Sst(0, (1, 512)), AF.Copy, [("A", "S")], [("A", "Sbf")])
                        else:
                            spdma(oret_p.ap()[l, h].rearrange("(c p) e -> p c e", p=128), Sst(0, (256, 2), (1, 256)),
                                  [("A", "S")], [], sem="Sst")
                        premask(j + 1, bs)
                    else:
                        o0_, l0_, r0_ = PS[bo](0, (1, 256)), scT(s_ * 128, (1, 128)), vtm(j * 256, (1, 256))
                        P.op("pe", lambda e, o0_=o0_, l0_=l0_, r0_=r0_: e.matmul(o0_, l0_, r0_, start=True, stop=False),
                             [("A", "scT", s_), ("A", "vtm")], [("ps", bo)])
                        for dc in range(2):
                            tt(qex(0, (136, 16), (1, 8)), qT(dc * T + 1024, (8, 16), (1, 8)),
                               TB(TC_QS + h * 128, (8, 16), (1, 8)), ALU.mult, [("A", "qT"), "tabs"], [("A", "qex")])
                            for g4 in range(4):
                                cs_ = (dc * 4 + g4) % 2
                                d_out = csb(cs_ * 1024, (256, 4), (1, 256))
                                d_in = sret_d.ap()[l, g4 * 4:g4 * 4 + 4, h, dc * 128:(dc + 1) * 128, :].rearrange("s p e -> p s e")
                                P.dma("pool", lambda e, d_out=d_out, d_in=d_in: e.dma_start(out=d_out, in_=d_in),
                                      ("csb", cs_), [], [("A", "csb", cs_)])
                                for s4 in range(4):
                                    s = g4 * 4 + s4
                                    o_, l_, r_ = PS[bo](0, (1, 256)), qex(s * 128, (1, 128)), csb(cs_ * 1024 + s4 * 256, (1, 256))
                                    last_ = (dc == 1 and s == 15)
                                    P.op("pe", lambda e, o_=o_, l_=l_, r_=r_, last_=last_: e.matmul(o_, l_, r_, start=False, stop=last_),
                                         [("A", "qex"), ("A", "csb", cs_)], [("ps", bo)])
                        gn3(j - 1)
                        gn1(j, bo)
                gn3(8)
                P.mark("L%dh%d_supd" % (l, h))
                def uload(u):
                    s, dc = u // 2, u % 2
                    spdma(usl((u % 3) * 256, (1, 256)), sret_d.ap()[l, s, h, dc * 128:(dc + 1) * 128, :],
                          [], [("A", "uin", u % 3)], sem=("uin", u % 3))
                for u in range(3):
                    uload(u)
                for u in range(32):
                    s, dc = u // 2, u % 2
                    k_ = s % 2
                    if dc == 0:
                        ts(kds(k_ * 256, (1, 256)), kdt(8 * 256, (1, 256)), TB(TC_OH + s, (1, 1)), None, ALU.mult, None,
                           [("A", "kdt"), "tabs"], [("A", "kds", k_)])
                    bu = cbank()
                    mm([(PS[bu](0, (1, 256)), [(kds(k_ * 256 + dc * 128, (1, 128)), vtm(8 * 256, (1, 256)))])],
                       [("A", "kds", k_), ("A", "vtm")], [("ps", bu)])
                    stt(usl(768 + (u % 3) * 256, (1, 256)), usl((u % 3) * 256, (1, 256)), g8, PS[bu](0, (1, 256)),
                        ALU.mult, ALU.add, [("A", "uin", u % 3), ("ps", bu)], [("A", "uout", u % 3)])
                    spdma(oret_s.ap()[l, s, h, dc * 128:(dc + 1) * 128, :], usl(768 + (u % 3) * 256, (1, 256)),
                          [("A", "uout", u % 3)], [], sem=("uout", u % 3))
                    if u + 3 < 32:
                        uload(u + 3)
                P.mark("L%dh%d_wout" % (l, h))
                wout_group(l, 1024 + h * 256, 2, mixc, ("A", "mixc"), tt_outer=False)
                P.mark("L%dh%d_end" % (l, h))

        def ple(l):
            pT = ab(36352)
            pin = af(40960)
            plt = af(43008)
            for j in range(NCH):
                s_ = j % 2
                spdma(pin(s_ * 256, (1, 256)), p_d.ap()[l, j * 128:(j + 1) * 128, :], [], [("A", "pin", s_)],
                      sem=("pin", s_))
                b = mbank()
                tr([(PS[b](i * 128, (1, 128)), pin(s_ * 256 + i * 128, (1, 128)), IDF) for i in range(2)],
                   [("A", "pin", s_), "tabs"], [("ps", b)])
                cp(pT(j * 128, (T, 2), (1, 128)), PS[b](0, (128, 2), (1, 128)), [("ps", b)], [("A", "pT")])
            for dc in range(16):
                wg, kg = wload(wtile("ple_gate", l, 0, 16, dc * 128), 16)
                wp, kp = wload(wtile("ple_proj", l, 0, 2, dc * 128), 2)
                for t in range(NTT):
                    ba, bb = nbank(), nbank()
                    mm([(PS[ba](0, (1, TW)), [(wg(kc), xb_rhs(kc, t * TW, TW)) for kc in range(16)])],
                       [kg, KX(t)], [("ps", ba)])
                    mm([(PS[bb](0, (1, TW)), [(wp(kc), pT(kc * T + t * TW, (1, TW))) for kc in range(2)])],
                       [kp, ("A", "pT")], [("ps", bb)])
                    s_ = t % 2
                    act(plt(s_ * TW, (1, TW)), PS[ba](0, (1, TW)), AF.Sigmoid, [("ps", ba)], [("A", "plt", s_)])
                    tt(plt(s_ * TW, (1, TW)), plt(s_ * TW, (1, TW)), PS[bb](0, (1, TW)), ALU.mult,
                       [("A", "plt", s_), ("ps", bb)], [("A", "plt", s_)])
                    xr = XR(dc * T + t * TW, (1, TW))
                    tt(xr, xr, plt(s_ * TW, (1, TW)), ALU.add, [("A", "plt", s_), KR(t)], [KR(t)])

        try:
            for l in range(NL):
                chk(1 + 20 * l)
                ffn(l, "ffn1")
                chk(2 + 20 * l)
                layernorm(l, 0)
                chk(3 + 20 * l)
                mixing(l)
                chk(10 + 20 * l)
                layernorm(l, 1)
                chk(11 + 20 * l)
                ffn(l, "ffn2")
                chk(12 + 20 * l)
                ple(l)
                chk(13 + 20 * l)
                layernorm(l, 2, final=(l == NL - 1))
        except _Stop:
            pass

        P.mark("out")
        P.fence(fence_nop)
        for j in range(NCH):
            sl = j % 2
            yo = af(sl * 8192)
            ky = ("A", "yo", sl)
            for q in range(4 if not KSUB & 4 else 0):
                b = mbank()
                tr([(PS[b](i * 128, (1, 128)), XR((q * 4 + i) * T + j * 128, (1, 128)), IDF) for i in range(4)],
                   [KR(j // 3), "tabs"], [("ps", b)])
                cp(yo(q * 512, (1, 512)), PS[b](0, (1, 512)), [("ps", b)], [ky], eng="act" if q % 2 else "dve")
            spdma(y_d.ap()[j * 128:(j + 1) * 128, :], yo(0, (1, 2048)), [ky], [], sem=("yo", sl))

        P.mark("end")
        _NC_CACHE["marks"] = P.marks
        P.emit()
    _NC_CACHE["used"] = set(I.keys()) | set(W.keys())
    _NC_CACHE["outs"] = set(O.keys())
    return nc


def _tables(hf):
    tab = np.zeros((128, NTAB), np.float64)
    pos = np.concatenate([hf * 1024 + np.arange(1024), np.tile(16384 + np.arange(8), 16)]).astype(np.float32)
    inv = (np.float32(10000.0) ** (-(np.arange(128, dtype=np.float32)) / np.float32(128))).astype(np.float32)
    ang = (pos[None, :] * inv[:, None]).astype(np.float32)
    tab[:, TC_COS:TC_COS + T] = np.cos(ang.astype(np.float64))
    tab[:, TC_SIN:TC_SIN + T] = np.sin(ang.astype(np.float64))
    m = np.arange(128)[:, None]
    lq = np.arange(128)[None, :]
    for h in range(4):
        g = GAM[h]
        mp = np.where(lq >= m, 0.0625 * g ** np.maximum(lq - m, 0).astype(np.float64), 0.0)
        tab[:, TC_MP + h * 128:TC_MP + (h + 1) * 128] = mp
        same = (m // 8) == (lq // 8)
        dj = (lq % 8) - (m % 8)
        ms = np.where(same & (dj >= 0), 0.0625 * g ** np.maximum(dj, 0).astype(np.float64), 0.0)
        tab[:, TC_MS + h * 128:TC_MS + (h + 1) * 128] = ms
        tab[:, TC_QP + h * 128:TC_QP + (h + 1) * 128] = (g ** (np.arange(128) + 1.0))[None, :]
        tab[:, TC_QS + h * 128:TC_QS + (h + 1) * 128] = (g ** ((np.arange(128) % 8) + 1.0))[None, :]
        for c in range(8):
            tab[:, TC_K1 + c * 4 + h] = 0.0625 * g ** (1023.0 - 128 * c - np.arange(128))
        tab[:, TC_K2 + h] = 0.0625 * g ** (127.0 - np.arange(128))
        tab[:, TC_KS + h] = 0.0625 * g ** (7.0 - (np.arange(128) % 8))
    for s in range(16):
        tab[:, TC_OH + s] = ((np.arange(128) // 8) == s).astype(np.float64)
    tab[:, TC_ISB] = float(hf)
    for gi in range(4):
        w = 2 << gi
        p16 = hf * 1024 + np.arange(16)
        tab[:, TC_CORR + gi * 16:TC_CORR + (gi + 1) * 16] = (w / np.minimum(w, p16 + 1.0))[None, :]
    tab[:, TC_ID:TC_ID + 128] = np.eye(128)
    tab[:, TC_ONES:TC_ONES + 128] = 1.0 / 2048.0
    return tab.astype(np.float32)


def kernel(x_prompt, x_sample, p_prompt, p_sample, cache_conv, cache_pool, state_ret,
           ln1_g, ln1_b, ffn1_w_gate, ffn1_w_up, ffn1_w_down, w_in, conv_w, pool_w, pool_scale,
           w_out, ln2_g, ln2_b, ffn2_w_gate, ffn2_w_up, ffn2_w_down, ple_gate, ple_proj, ln3_g, ln3_b):
    f = lambda a: np.ascontiguousarray(np.asarray(a, dtype=np.float32))
    x_prompt, x_sample, p_prompt, p_sample = f(x_prompt), f(x_sample), f(p_prompt), f(p_sample)
    cache_conv, cache_pool, state_ret = f(cache_conv), f(cache_pool), f(state_ret)
    vecs = np.zeros((128, NV), np.float32)
    for l in range(NL):
        base = l * 112
        for k, a in enumerate((ln1_g, ln1_b, ln2_g, ln2_b, ln3_g, ln3_b)):
            vecs[:, base + k * 16:base + (k + 1) * 16] = f(a)[l].reshape(16, 128).T
        cw = f(conv_w)[l]
        for ci in range(4):
            for j in range(3):
                vecs[:, base + 96 + ci * 3 + j] = cw[j, ci * 128:(ci + 1) * 128]
        vecs[:, base + 108:base + 112] = f(pool_scale)[l].reshape(4, 128).T
    wts = {"ffn1_w_gate": f(ffn1_w_gate), "ffn1_w_up": f(ffn1_w_up), "ffn1_w_down": f(ffn1_w_down),
           "w_in": f(w_in), "pool_w": f(pool_w), "w_out": f(w_out),
           "ffn2_w_gate": f(ffn2_w_gate), "ffn2_w_up": f(ffn2_w_up), "ffn2_w_down": f(ffn2_w_down),
           "ple_gate": f(ple_gate), "ple_proj": f(ple_proj)}
    tabs = [_tables(0), _tables(1)]
    in_maps = []
    for c in range(8):
        pr, hf = c // 2, c % 2
        sl = slice(16 * c, 16 * c + 16)
        m = {
            "x": np.concatenate([x_prompt[pr, hf * 1024:(hf + 1) * 1024], x_sample[sl].reshape(128, D)], 0),
            "p": np.concatenate([p_prompt[:, pr, hf * 1024:(hf + 1) * 1024], p_sample[:, sl].reshape(NL, 128, 256)], 1),
            "cconv": cache_conv[:, sl].reshape(NL, 32, 512),
            "cpool": cache_pool[:, sl].reshape(NL, 240, 512),
            "sret": state_ret[:, sl],
            "vecs": vecs, "tabs": tabs[hf],
        }
        m = {k: np.ascontiguousarray(v) for k, v in m.items()}
        m.update(wts)
        in_maps.append(m)
    if os.environ.get("KDBG_MAPS"):
        return in_maps
    if "nc" not in _NC_CACHE:
        _NC_CACHE["nc"] = build_program()
    used = _NC_CACHE["used"]
    in_maps = [{k: v for k, v in m.items() if k in used} for m in in_maps]
    res = run_bass_kernel_spmd(_NC_CACHE["nc"], in_maps, core_ids=list(range(8)))
    R = res.results
    y_prompt = np.zeros((4, 2048, D), np.float32)
    y_sample = np.zeros((128, 8, D), np.float32)
    conv_p = np.zeros((NL, 4, 2, 512), np.float32)
    pool_p = np.zeros((NL, 4, 15, 512), np.float32)
    ret_p = np.zeros((NL, 4, 4, 256, 256), np.float32)
    conv_s = np.zeros((NL, 128, 2, 512), np.float32)
    pool_s = np.zeros((NL, 128, 15, 512), np.float32)
    ret_s = np.zeros((NL, 128, 4, 256, 256), np.float32)
    for c in range(8):
        pr, hf = c // 2, c % 2
        sl = slice(16 * c, 16 * c + 16)
        r = R[c]
        y = np.asarray(r["y"])
        y_prompt[pr, hf * 1024:(hf + 1) * 1024] = y[:1024]
        y_sample[sl] = y[1024:].reshape(16, 8, D)
        if "oconv_s" in r:
            conv_s[:, sl] = np.asarray(r["oconv_s"]).reshape(NL, 16, 2, 512)
        if "opool_s" in r:
            pool_s[:, sl] = np.asarray(r["opool_s"]).reshape(NL, 16, 15, 512)
        if "oret_s" in r:
            ret_s[:, sl] = np.asarray(r["oret_s"])
        if hf == 1:
            if "oconv_p" in r:
                conv_p[:, pr] = np.asarray(r["oconv_p"])
            if "opool_p" in r:
                pool_p[:, pr] = np.asarray(r["opool_p"])
            if "oret_p" in r:
                ret_p[:, pr] = np.asarray(r["oret_p"])
    return (y_prompt, y_sample, conv_p, pool_p, ret_p, conv_s, pool_s, ret_s)
```

```python
import math
import os
import contextlib
import numpy as np
import concourse.bass as bass
import concourse.mybir as mybir
from concourse.bass_utils import run_bass_kernel_spmd

F32 = mybir.dt.float32
BF16 = mybir.dt.bfloat16
AF = mybir.ActivationFunctionType
ALU = mybir.AluOpType

D = 2048
FF = 5632
NL = 2
T = 1152
TPR = 1024
NTT = 3
TW = 384
NCH = 9
ALPHA = 4 ** 0.25
EPS = 1e-5
GAM = [1.0 - 2.0 ** (-5 - h) for h in range(4)]
NSLOT = 4
ARENA = 64512
NV = 224
TC_COS, TC_SIN, TC_MP, TC_MS, TC_QP, TC_QS = 0, 1152, 2304, 2816, 3328, 3840
TC_K1, TC_K2, TC_KS, TC_OH, TC_ISB, TC_CORR, TC_ID, TC_ONES = 4352, 4384, 4388, 4392, 4408, 4416, 4480, 4608
NTAB = 4736


class _Op:
    __slots__ = ("eng", "fn", "deps", "is_dma", "dma_sem", "dma_val", "signal", "count", "idx", "inc", "near")


SMALL_KEYS = {"stat", "stat3", "xs1", "xr1", "cvt", "pvt", "mean", "rstd", "corr"}


class Prog:
    ENGS = ("pe", "act", "dve", "pool", "sp")

    def __init__(self, nc):
        self.nc = nc
        self.ops = []
        self.last_w = {}
        self.readers = {}
        self.dma_sems = {}
        self.fence_op = None
        self.pe_n = 0
        self.marks = []

    def _add(self, eng, fn, reads, writes, is_dma, dma_sem, inc=16):
        op = _Op()
        op.eng, op.fn, op.is_dma, op.dma_sem = eng, fn, is_dma, dma_sem
        op.signal = False
        op.count = 0
        op.inc = inc
        op.idx = len(self.ops)
        op.near = any(isinstance(k, tuple) and len(k) > 1 and k[0] == "A" and k[1] in SMALL_KEYS
                      for k in list(reads) + list(writes))
        deps = set()
        for k in list(reads) + list(writes):
            if (self.fence_op is not None and isinstance(k, tuple) and k and k[0] == "A"
                    and k not in self.last_w and k not in self.readers):
                deps.add(self.fence_op)
        for r in reads:
            w = self.last_w.get(r)
            if w is not None:
                deps.add(w)
        for w_ in writes:
            w = self.last_w.get(w_)
            if w is not None:
                deps.add(w)
            for rd in self.readers.get(w_, ()):
                deps.add(rd)
        for r in reads:
            if isinstance(r, tuple) and r and r[0] in ("ps", "ps7"):
                for rd in self.readers.get(r, ()):
                    deps.add(rd)
        op.deps = deps
        for r in reads:
            self.readers.setdefault(r, []).append(op.idx)
        for w_ in writes:
            self.last_w[w_] = op.idx
            self.readers[w_] = []
        if is_dma:
            ent = self.dma_sems.setdefault(dma_sem, [None, 0])
            ent[1] += inc
            op.dma_val = ent[1]
        self.ops.append(op)
        return op

    def op(self, eng, fn, reads=(), writes=(), n=1):
        if eng == "pe":
            self.pe_n += n
        return self._add(eng, fn, reads, writes, False, None)

    def mark(self, label):
        self.marks.append((label, self.pe_n))

    def dma(self, eng, fn, sem, reads=(), writes=(), inc=16):
        return self._add(eng, fn, reads, writes, True, sem, inc)

    def fence(self, fn):
        akeys = [k for k in set(list(self.last_w) + list(self.readers))
                 if isinstance(k, tuple) and k and k[0] == "A"]
        op = self._add("dve", fn, akeys, akeys, False, None)
        for k in akeys:
            self.last_w.pop(k, None)
            self.readers.pop(k, None)
        self.fence_op = op.idx

    def emit(self):
        nc = self.nc
        ops = self.ops
        epos = {e: 0 for e in self.ENGS}
        for op in ops:
            epos[op.eng] += 1
            op.count = epos[op.eng]
        pos = {op.idx: op.count for op in ops}
        self._pos = pos
        NEAR = 6

        def same_eng_skip(op, dop):
            if dop.eng != op.eng or op.is_dma:
                return False
            if (op.near or dop.near) and op.eng in ("act", "dve", "pool") and pos[op.idx] - pos[dop.idx] <= NEAR:
                return False
            return True
        self._skip = same_eng_skip
        for op in ops:
            for d in op.deps:
                dop = ops[d]
                if dop.is_dma:
                    continue
                if same_eng_skip(op, dop):
                    continue
                dop.signal = True
        cnt = {e: 0 for e in self.ENGS}
        for op in ops:
            if op.signal:
                cnt[op.eng] += 1
            op.count = cnt[op.eng]
        by_eng = {e: [o for o in ops if o.eng == e] for e in self.ENGS}
        with contextlib.ExitStack() as st:
            esem = {e: st.enter_context(nc.semaphore("sem_" + e)) for e in self.ENGS}
            for i, (name, ent) in enumerate(self.dma_sems.items()):
                ent[0] = st.enter_context(nc.semaphore("dsem_%d" % i))
            block = st.enter_context(nc.Block())
            dma_final = [(ent[0], ent[1]) for ent in self.dma_sems.values()]

            def run(ename, e):
                waited = {}
                for op in by_eng[ename]:
                    need = {}
                    for d in op.deps:
                        dop = ops[d]
                        if dop.is_dma:
                            key = ("d", dop.dma_sem)
                            sem, val = self.dma_sems[dop.dma_sem][0], dop.dma_val
                        else:
                            if self._skip(op, dop):
                                continue
                            key = ("e", dop.eng)
                            sem, val = esem[dop.eng], dop.count
                        if need.get(key, (None, -1))[1] < val:
                            need[key] = (sem, val)
                    for key, (sem, val) in need.items():
                        if waited.get(key, -1) >= val:
                            continue
                        e.wait_ge(sem, val)
                        waited[key] = val
                    ins = op.fn(e)
                    if op.is_dma:
                        ins.then_inc(self.dma_sems[op.dma_sem][0], op.inc)
                    elif op.signal:
                        ins.then_inc(esem[ename], 1)
                if ename == "sp":
                    for sem, val in dma_final:
                        e.wait_ge(sem, val)
                    for en in self.ENGS:
                        if en != "sp" and cnt[en] > 0:
                            e.wait_ge(esem[en], cnt[en])

            @block.tensor
            def _(e):
                run("pe", e)

            @block.scalar
            def _(e):
                run("act", e)

            @block.vector
            def _(e):
                run("dve", e)

            @block.gpsimd
            def _(e):
                run("pool", e)

            @block.sync
            def _(e):
                run("sp", e)


class Buf:
    def __init__(self, ap2d, off=0):
        self.t = ap2d.tensor
        self.base = ap2d.offset + off
        self.ps = ap2d.ap[0][0]

    def __call__(self, off, *dims, p0=0, pn=128):
        return bass.AP(self.t, self.base + p0 * self.ps + off, [[self.ps, pn]] + [list(d) for d in dims])


_NC_CACHE = {}


def build_program():
    nc = bass.Bass("TRN2", target_bir_lowering=False)
    dt_in = lambda n, s: nc.dram_tensor(n, s, F32, kind="ExternalInput")
    dt_out = lambda n, s: nc.dram_tensor(n, s, F32, kind="ExternalOutput")
    ISH = {"x": [T, D], "p": [NL, T, 256], "cconv": [NL, 32, 512], "cpool": [NL, 240, 512],
           "sret": [NL, 16, 4, 256, 256], "vecs": [128, NV], "tabs": [128, NTAB]}

    class _LazyI(dict):
        def __missing__(self, nm):
            self[nm] = dt_in(nm, ISH[nm])
            return self[nm]
    I = _LazyI()

    class _L:
        def __init__(self, nm):
            self.nm = nm

        def ap(self):
            return I[self.nm].ap()
    x_d, p_d, cconv_d, cpool_d, sret_d, vecs_d, tabs_d = (_L(n) for n in ("x", "p", "cconv", "cpool", "sret", "vecs", "tabs"))
    WSH = {"ffn1_w_gate": [NL, D, FF], "ffn1_w_up": [NL, D, FF], "ffn1_w_down": [NL, FF, D],
           "w_in": [NL, D, 6144], "pool_w": [NL, 4, 128, 128], "w_out": [NL, D, D],
           "ffn2_w_gate": [NL, D, FF], "ffn2_w_up": [NL, D, FF], "ffn2_w_down": [NL, FF, D],
           "ple_gate": [NL, D, D], "ple_proj": [NL, 256, D]}

    class _LazyW(dict):
        def __missing__(self, nm):
            self[nm] = dt_in(nm, WSH[nm])
            return self[nm]
    W = _LazyW()
    OSH = {"y": [T, D], "oconv_s": [NL, 32, 512], "opool_s": [NL, 240, 512], "oret_s": [NL, 16, 4, 256, 256],
           "oconv_p": [NL, 2, 512], "opool_p": [NL, 15, 512], "oret_p": [NL, 4, 256, 256]}

    class _LazyO(dict):
        def __missing__(self, nm):
            self[nm] = dt_out(nm, OSH[nm])
            return self[nm]
    O = _LazyO()

    class _LO:
        def __init__(self, nm):
            self.nm = nm

        def ap(self):
            return O[self.nm].ap()
    y_d, oconv_s, opool_s, oret_s, oconv_p, opool_p, oret_p = (
        _LO(n) for n in ("y", "oconv_s", "opool_s", "oret_s", "oconv_p", "opool_p", "oret_p"))

    class _LazyC(dict):
        def __missing__(self, key):
            nm, l = key
            shp = {"cc1_in": [128, 72], "cc1_out": [256, 72], "cc2_in": [128, 2048], "cc2_out": [256, 2048]}[nm]
            self[key] = nc.dram_tensor("%s%d" % (nm, l), shp, F32)
            return self[key]
    CC = _LazyC()

    class _LC:
        def __init__(self, nm):
            self.nm = nm

        def __getitem__(self, l):
            return CC[(self.nm, l)]
    cc1_in, cc1_out, cc2_in, cc2_out = _LC("cc1_in"), _LC("cc1_out"), _LC("cc2_in"), _LC("cc2_out")
    PAIRS = [[0, 1], [2, 3], [4, 5], [6, 7]]

    P = Prog(nc)
    stage = int(os.environ.get("KSTAGE", "999"))

    class _Stop(Exception):
        pass

    def chk(n):
        P.mark("stage%d" % n)
        if n > stage:
            raise _Stop()

    with contextlib.ExitStack() as st:
        sbt = lambda n, s, d: st.enter_context(nc.sbuf_tensor("sb_" + n, s, d))
        xres_t = sbt("xres", [128, 16 * T], F32)
        xbf_t = sbt("xbf", [128, 16 * T], BF16)
        tabs_t = sbt("tabs", [128, NTAB], F32)
        vecs_t = sbt("vecs", [128, NV], F32)
        valp_t = sbt("valp", [128, NV], F32)
        idb_t = sbt("idb", [128, 128], BF16)
        wsl_t = sbt("wsl", [128, NSLOT * 2048], BF16)
        ar_t = sbt("arena", [128, ARENA // 4], F32)
        psb = [st.enter_context(nc.psum_tensor("ps%d" % i, [128, 512], F32)) for i in range(6)]
        ps7_t = [st.enter_context(nc.psum_tensor("psb%d" % i, [128, 1024], BF16)) for i in range(2)]

        XR = Buf(xres_t[:, :])
        XB = Buf(xbf_t[:, :])
        TB = Buf(tabs_t[:, :])
        VC = Buf(vecs_t[:, :])
        VA = Buf(valp_t[:, :])
        IDB = Buf(idb_t[:, :])
        WS = Buf(wsl_t[:, :])
        AF32 = Buf(ar_t[:, :])
        ABF = Buf(ar_t[:, :].bitcast(BF16))
        PS = [Buf(b[:, :]) for b in psb]
        PS7L = [Buf(t_[:, :]) for t_ in ps7_t]

        def af(off_b):
            return Buf(ar_t[:, :], off_b // 4)

        def ab(off_b):
            return Buf(ar_t[:, :].bitcast(BF16), off_b // 2)

        IDF = TB(TC_ID, (1, 128))
        ONES = TB(TC_ONES, (1, 128))

        bank_ctr = [0]

        def nbank():
            b = bank_ctr[0] % 4
            bank_ctr[0] += 1
            return b

        misc_ctr = [0]

        def mbank():
            b = 4 + misc_ctr[0] % 2
            misc_ctr[0] += 1
            return b

        h7_ctr = [0]

        def h7():
            b = h7_ctr[0] % 2
            h7_ctr[0] += 1
            return b

        def mm(groups, reads, writes):
            def fn(e):
                ins = None
                for out, pairs in groups:
                    n = len(pairs)
                    for i, (l, r) in enumerate(pairs):
                        ins = e.matmul(out, l, r, start=(i == 0), stop=(i == n - 1))
                return ins
            P.op("pe", fn, reads, writes, n=sum(len(p_) for _, p_ in groups))

        def tr(items, reads, writes):
            def fn(e):
                ins = None
                for o, i_, idn in items:
                    ins = e.transpose(o, i_, idn)
                return ins
            P.op("pe", fn, reads, writes, n=len(items))

        def act(out, in_, func, reads, writes, bias=None, scale=None):
            kw = {}
            if bias is not None:
                kw["bias"] = bias
            if scale is not None:
                kw["scale"] = scale
            P.op("act", lambda e: e.activation(out, in_, func, **kw), reads, writes)

        def tt(out, in0, in1, op, reads, writes, eng="dve"):
            P.op(eng, lambda e: e.tensor_tensor(out, in0, in1, op), reads, writes)

        def ts(out, in0, s1, s2, op0, op1, reads, writes, eng="dve"):
            if s2 is None:
                P.op(eng, lambda e: e.tensor_scalar(out, in0, s1, None, op0), reads, writes)
            else:
                P.op(eng, lambda e: e.tensor_scalar(out, in0, s1, s2, op0, op1), reads, writes)

        def stt(out, in0, sc, in1, op0, op1, reads, writes, eng="dve"):
            P.op(eng, lambda e: e.scalar_tensor_tensor(out, in0, sc, in1, op0, op1), reads, writes)

        def cp(out, in_, reads, writes, eng="dve"):
            if eng == "act":
                P.op(eng, lambda e: e.activation(out, in_, AF.Copy), reads, writes)
            else:
                P.op(eng, lambda e: e.tensor_copy(out, in_), reads, writes)

        sp_ctr = [0]

        def spdma(out, in_, reads, writes, sem=None):
            if sem is None:
                sem = ("sp", sp_ctr[0] % 8)
                sp_ctr[0] += 1
            P.dma("sp", lambda e: e.dma_start(out=out, in_=in_), sem, reads, writes)

        ws_ctr = [0]

        def wload(dram_ap, nk):
            s = ws_ctr[0] % NSLOT
            ws_ctr[0] += 1
            key = ("w", s)
            out = WS(s * 2048, (128, nk), (1, 128))
            P.dma("pool", lambda e: e.dma_start(out=out, in_=dram_ap), ("w", s), (), [key])
            return (lambda kc, s=s: WS(s * 2048 + kc * 128, (1, 128))), key

        def wtile(name, l, r0, nk, c0):
            return W[name].ap()[l][r0:r0 + nk * 128, c0:c0 + 128].rearrange("(k p) c -> p k c", p=128)

        def xb_rhs(kc, t0, n):
            return XB(kc * T + t0, (1, n))

        KX = lambda t: ("xbf", t)
        KR = lambda t: ("xres", t)

        spdma(tabs_t[:, :], tabs_d.ap(), (), ["tabs"], sem="c0")
        spdma(vecs_t[:, :], vecs_d.ap(), (), ["vecs"], sem="c1")
        KSUB = int(os.environ.get("KSUB", "0"))
        if not KSUB & 1:
            ts(valp_t[:, :], vecs_t[:, :], float(ALPHA), None, ALU.mult, None, ["vecs"], ["valp"])
        if not KSUB & 8:
            cp(idb_t[:, :], IDF, ["tabs"], ["idb"])

        for j in range(NCH if not KSUB & 2 else 0):
            sl = j % 2
            xin = af(sl * 8192)
            kx = ("A", "xin", sl)
            spdma(xin(0, (1, 2048)), x_d.ap()[j * 128:(j + 1) * 128, :], (), [kx], sem=("xin", sl))
            for q in range(4):
                b = mbank()
                tr([(PS[b](i * 128, (1, 128)), xin((q * 4 + i) * 128, (1, 128)), IDF) for i in range(4)],
                   [kx, "tabs"], [("ps", b)])
                o_r = XR(q * 4 * T + j * 128, (T, 4), (1, 128))
                o_b = XB(q * 4 * T + j * 128, (T, 4), (1, 128))
                src = PS[b](0, (128, 4), (1, 128))
                if not KSUB & 16:
                    act(o_r, src, AF.Identity, [("ps", b)], [KR(j // 3)], scale=float(ALPHA))
                if not KSUB & 32:
                    cp(o_b, src, [("ps", b)], [KX(j // 3)])

        fence_nop = lambda e: e.tensor_copy(valp_t[:, 0:1], valp_t[:, 0:1])

        def ffn(l, pre):
            P.fence(lambda e: e.tensor_copy(valp_t[:, 0:1], valp_t[:, 0:1]))
            hb = ab(0)
            sg = af(25344)
            sgc = [0]
            for g in range(4):
                for fi in range(11):
                    ft = g * 11 + fi
                    wg, kg = wload(wtile(pre + "_w_gate", l, 0, 16, ft * 128), 16)
                    wu, ku = wload(wtile(pre + "_w_up", l, 0, 16, ft * 128), 16)
                    for t in range(NTT):
                        ba, bb = nbank(), nbank()
                        mm([(PS[ba](0, (1, TW)), [(wg(kc), xb_rhs(kc, t * TW, TW)) for kc in range(16)])],
                           [kg, KX(t)], [("ps", ba)])
                        mm([(PS[bb](0, (1, TW)), [(wu(kc), xb_rhs(kc, t * TW, TW)) for kc in range(16)])],
                           [ku, KX(t)], [("ps", bb)])
                        s_ = sgc[0] % 3
                        sgc[0] += 1
                        ksg = ("A", "sg", s_)
                        act(sg(s_ * TW, (1, TW)), PS[ba](0, (1, TW)), AF.Silu, [("ps", ba)], [ksg])
                        tt(hb(fi * T + t * TW, (1, TW)), sg(s_ * TW, (1, TW)), PS[bb](0, (1, TW)), ALU.mult,
                           [ksg, ("ps", bb)], [("A", "h", t)])
                def down(dc, t, wd, kd):
                    b = nbank()
                    mm([(PS[b](0, (1, TW)), [(wd(fi), hb(fi * T + t * TW, (1, TW))) for fi in range(11)])],
                       [kd, ("A", "h", t)], [("ps", b)])
                    xr = XR(dc * T + t * TW, (1, TW))
                    stt(xr, PS[b](0, (1, TW)), 0.5, xr, ALU.mult, ALU.add, [("ps", b), KR(t)], [KR(t)])
                if True:
                    for dc in range(16):
                        wd, kd = wload(wtile(pre + "_w_down", l, g * 1408, 11, dc * 128), 11)
                        for t in range(NTT):
                            down(dc, t, wd, kd)
                else:
                    for t in range(NTT):
                        for dc in range(16):
                            wd, kd = wload(wtile(pre + "_w_down", l, g * 1408, 11, dc * 128), 11)
                            down(dc, t, wd, kd)

        def layernorm(l, which, final=False):
            gcol = l * 112 + which * 32
            bcol = gcol + 16
            P.fence(fence_nop)
            mean = af(30208)
            rstd = af(30208 + 1536)
            sq = af(30208 + 3072)
            for t in range(NTT):
                kt = KR(t)
                b = mbank()
                mm([(PS[b](0, (1, TW)), [(ONES, XR(dc * T + t * TW, (1, TW))) for dc in range(16)])],
                   [kt, "tabs"], [("ps", b)])
                cp(mean(0, (1, TW)), PS[b](0, (1, TW)), [("ps", b)], [("A", "mean")])
                xr3 = XR(t * TW, (T, 16), (1, TW))
                tt(xr3, xr3, mean(0, (0, 16), (1, TW)), ALU.subtract, [kt, ("A", "mean")], [kt])
                b2 = mbank()
                for dc in range(16):
                    s_ = dc % 2
                    act(sq(s_ * TW, (1, TW)), XR(dc * T + t * TW, (1, TW)), AF.Square, [kt], [("A", "sq", s_)])
                    o = PS[b2](0, (1, TW))
                    l_, r_ = ONES, sq(s_ * TW, (1, TW))
                    P.op("pe", (lambda e, o=o, l_=l_, r_=r_, dc=dc: e.matmul(o, l_, r_, start=(dc == 0), stop=(dc == 15))),
                         [("A", "sq", s_), "tabs"], [("ps", b2)])
                act(rstd(0, (1, TW)), PS[b2](0, (1, TW)), AF.Sqrt, [("ps", b2)], [("A", "rstd")], bias=float(EPS))
                P.op("dve", lambda e: e.reciprocal(rstd(0, (1, TW)), rstd(0, (1, TW))), [("A", "rstd")], [("A", "rstd")])
                tt(xr3, xr3, rstd(0, (0, 16), (1, TW)), ALU.mult, [kt, ("A", "rstd")], [kt])
                for dc in range(16):
                    xr = XR(dc * T + t * TW, (1, TW))
                    if not final:
                        act(XB(dc * T + t * TW, (1, TW)), xr, AF.Identity, [kt, "vecs"], [KX(t)],
                            bias=VC(bcol + dc, (1, 1)), scale=VC(gcol + dc, (1, 1)))
                        act(xr, xr, AF.Identity, [kt, "valp"], [kt],
                            bias=VA(bcol + dc, (1, 1)), scale=VA(gcol + dc, (1, 1)))
                    else:
                        act(xr, xr, AF.Identity, [kt, "vecs"], [kt],
                            bias=VC(bcol + dc, (1, 1)), scale=VC(gcol + dc, (1, 1)))

        def wout_group(l, r0, nk, mixbuf, mixkey, tt_outer=False):
            def one(dc, t, w, kw):
                b = nbank()
                mm([(PS[b](0, (1, TW)), [(w(kc), mixbuf(kc * T + t * TW, (1, TW))) for kc in range(nk)])],
                   [kw, mixkey], [("ps", b)])
                xr = XR(dc * T + t * TW, (1, TW))
                tt(xr, PS[b](0, (1, TW)), xr, ALU.add, [("ps", b), KR(t)], [KR(t)])
            if not tt_outer:
                for dc in range(16):
                    w, kw = wload(wtile("w_out", l, r0, nk, dc * 128), nk)
                    for t in range(NTT):
                        one(dc, t, w, kw)
            else:
                for t in range(NTT):
                    for dc in range(16):
                        w, kw = wload(wtile("w_out", l, r0, nk, dc * 128), nk)
                        one(dc, t, w, kw)

        def zmm(l, col0, evac):
            w, kw = wload(wtile("w_in", l, 0, 16, col0), 16)
            for t in range(NTT):
                b = nbank()
                mm([(PS[b](0, (1, TW)), [(w(kc), xb_rhs(kc, t * TW, TW)) for kc in range(16)])],
                   [kw, KX(t)], [("ps", b)])
                evac(t, b)

        def pair_mm(l, colA, colB, evac):
            w1, k1 = wload(wtile("w_in", l, 0, 16, colA), 16)
            w2, k2 = wload(wtile("w_in", l, 0, 16, colB), 16)
            for t in range(NTT):
                b1, b2 = nbank(), nbank()
                mm([(PS[b1](0, (1, TW)), [(w1(kc), xb_rhs(kc, t * TW, TW)) for kc in range(16)])],
                   [k1, KX(t)], [("ps", b1)])
                mm([(PS[b2](0, (1, TW)), [(w2(kc), xb_rhs(kc, t * TW, TW)) for kc in range(16)])],
                   [k2, KX(t)], [("ps", b2)])
                evac(t, b1, b2)

        def rot_pair(l, col0, outbuf, outkey, rt):
            def ev(t, b1, b2):
                cs = TB(TC_COS + t * TW, (1, TW))
                sn = TB(TC_SIN + t * TW, (1, TW))
                p1, p2 = PS[b1](0, (1, TW)), PS[b2](0, (1, TW))
                t1, t2 = rt(0, (1, TW)), rt(TW, (1, TW))
                ka, kb = ("A", "rt", 0), ("A", "rt", 1)
                tt(t1, p1, cs, ALU.mult, [("ps", b1), "tabs"], [ka])
                tt(t2, p2, sn, ALU.mult, [("ps", b2), "tabs"], [kb])
                tt(outbuf(t * TW, (1, TW)), t1, t2, ALU.subtract, [ka, kb], [outkey])
                tt(t1, p1, sn, ALU.mult, [("ps", b1), "tabs"], [ka])
                tt(t2, p2, cs, ALU.mult, [("ps", b2), "tabs"], [kb])
                tt(outbuf(T + t * TW, (1, TW)), t1, t2, ALU.add, [ka, kb], [outkey])
            pair_mm(l, col0, col0 + 128, ev)

        def mixing(l):
            P.fence(fence_nop)
            UW, PW = 1186, 1407
            uf = af(0)
            zp = af(18976)
            zc = af(41488)
            mixb = ab(46096)
            cst = af(55312)
            xs1 = af(59408)
            KU = lambda ci: ("A", "uf", ci)
            KZ = lambda gi: ("A", "zp", gi)
            vb = l * 112

            spdma(cst(0, (1, 512), pn=32), cconv_d.ap()[l], (), [("A", "cst")], sem="cst")
            for ci in range(4):
                b = mbank()
                tr([(PS[b](0, (1, 32)), cst(ci * 128, (1, 128), pn=32), TB(TC_ID, (1, 32), pn=32))],
                   [("A", "cst"), "tabs"], [("ps", b)])
                cp(uf(ci * UW + 1026, (10, 16), (1, 2)), PS[b](0, (2, 16), (1, 2)), [("ps", b)], [KU(ci)])
            for half, (r0, nr) in enumerate(((0, 128), (128, 112))):
                spdma(cst(0, (1, 512), pn=nr), cpool_d.ap()[l][r0:r0 + nr, :], (), [("A", "cst")], sem="cst")
                for gi in range(4):
                    b = mbank()
                    tr([(PS[b](0, (1, nr)), cst(gi * 128, (1, 128), pn=nr), TB(TC_ID, (1, nr), pn=nr))],
                       [("A", "cst"), "tabs"], [("ps", b)])
                    if half == 0:
                        cp(zp(gi * PW + 1039, (23, 8), (1, 15)), PS[b](0, (15, 8), (1, 15)), [("ps", b)], [KZ(gi)])
                        cp(zp(gi * PW + 1039 + 8 * 23, (1, 8)), PS[b](120, (1, 8)), [("ps", b)], [KZ(gi)])
                    else:
                        cp(zp(gi * PW + 1039 + 8 * 23 + 8, (1, 7)), PS[b](0, (1, 7)), [("ps", b)], [KZ(gi)])
                        cp(zp(gi * PW + 1039 + 9 * 23, (23, 7), (1, 15)), PS[b](7, (15, 7), (1, 15)),
                           [("ps", b)], [KZ(gi)])

            for ci in range(4):
                def ev_c(t, b):
                    act(zc(t * TW, (1, TW)), PS[b](0, (1, TW)), AF.Copy, [("ps", b)], [("A", "zc")])
                zmm(l, 512 + ci * 128, ev_c)

                def ev_h(t, b, ci=ci):
                    if t < 2:
                        tt(uf(ci * UW + 2 + t * TW, (1, TW)), PS[b](0, (1, TW)), zc(t * TW, (1, TW)), ALU.mult,
                           [("ps", b), ("A", "zc")], [KU(ci)])
                    else:
                        tt(uf(ci * UW + 2 + 768, (1, 256)), PS[b](0, (1, 256)), zc(768, (1, 256)), ALU.mult,
                           [("ps", b), ("A", "zc")], [KU(ci)])
                        tt(uf(ci * UW + 1028, (10, 16), (1, 8)), PS[b](256, (8, 16), (1, 8)),
                           zc(1024, (8, 16), (1, 8)), ALU.mult, [("ps", b), ("A", "zc")], [KU(ci)])
                zmm(l, 1024 + ci * 128, ev_h)
            for gi in range(4):
                def ev_p(t, b, gi=gi):
                    if t < 2:
                        act(zp(gi * PW + 15 + t * TW, (1, TW)), PS[b](0, (1, TW)), AF.Copy, [("ps", b)], [KZ(gi)])
                    else:
                        act(zp(gi * PW + 15 + 768, (1, 256)), PS[b](0, (1, 256)), AF.Copy, [("ps", b)], [KZ(gi)])
                        act(zp(gi * PW + 1039 + 15, (23, 16), (1, 8)), PS[b](256, (8, 16), (1, 8)), AF.Copy,
                            [("ps", b)], [KZ(gi)])
                zmm(l, 1536 + gi * 128, ev_p)

            chk(4 + 20 * l)
            allu = [KU(i) for i in range(4)]
            allz = [KZ(i) for i in range(4)]
            cp(xs1(0, (2, 4), (1, 2)), uf(1024, (UW, 4), (1, 2)), allu, [("A", "xs1")])
            cp(xs1(8, (15, 4), (1, 15)), zp(1024, (PW, 4), (1, 15)), allz, [("A", "xs1")])
            P.op("dve", lambda e: e.memset(xs1(68, (1, 4)), 0.0), [], [("A", "xs1")])
            P.dma("pool", lambda e: e.dma_start(out=cc1_in[l].ap(), in_=xs1(0, (1, 72))), ("cc1i", l),
                  [("A", "xs1")], [("cc1in", l)])
            NOCC = bool(int(os.environ.get("KSUB", "0")) & 64)
            if not NOCC:
                P.dma("pool", lambda e: e.collective_compute("AllGather", ALU.bypass, replica_groups=PAIRS,
                                                             ins=[cc1_in[l].ap().opt()], outs=[cc1_out[l].ap().opt()]),
                      ("cc1", l), [("cc1in", l)], [("cc1out", l)], inc=1)
                P.dma("pool", lambda e: e.dma_start(out=xs1(72, (1, 72)), in_=cc1_out[l].ap()[0:128, :]), ("cc1o", l),
                      [("cc1out", l)], [("A", "xr1")])
            else:
                P.dma("pool", lambda e: e.dma_start(out=xs1(72, (1, 72)), in_=cc1_in[l].ap()), ("cc1o", l),
                      [("cc1in", l)], [("A", "xr1")])
            isb = TB(TC_ISB, (1, 1))
            ts(uf(0, (UW, 4), (1, 2)), xs1(72, (2, 4), (1, 2)), isb, None, ALU.mult, None,
               [("A", "xr1"), "tabs"], allu)
            ts(zp(0, (PW, 4), (1, 15)), xs1(80, (15, 4), (1, 15)), isb, None, ALU.mult, None,
               [("A", "xr1"), "tabs"], allz)

            chk(5 + 20 * l)
            for ci in range(4):
                wc = lambda j, ci=ci: VC(vb + 96 + ci * 3 + j, (1, 1))
                kk = [KU(ci), "vecs"]
                ts(zc(0, (1, 1024)), uf(ci * UW, (1, 1024)), wc(0), None, ALU.mult, None, kk, [("A", "zc")])
                ts(zc(1024, (8, 16), (1, 8)), uf(ci * UW + 1026, (10, 16), (1, 8)), wc(0), None, ALU.mult, None,
                   kk, [("A", "zc")])
                for j in (1, 2):
                    stt(zc(0, (1, 1024)), uf(ci * UW + j, (1, 1024)), wc(j), zc(0, (1, 1024)), ALU.mult, ALU.add,
                        kk + [("A", "zc")], [("A", "zc")])
                    stt(zc(1024, (8, 16), (1, 8)), uf(ci * UW + 1026 + j, (10, 16), (1, 8)), wc(j),
                        zc(1024, (8, 16), (1, 8)), ALU.mult, ALU.add, kk + [("A", "zc")], [("A", "zc")])

                def ev_b(t, b, ci=ci):
                    tt(mixb(ci * T + t * TW, (1, TW)), PS[b](0, (1, TW)), zc(t * TW, (1, TW)), ALU.mult,
                       [("ps", b), ("A", "zc")], [("A", "mix")])
                zmm(l, ci * 128, ev_b)
            cvt = af(59408 + 576)
            cp(cvt(0, (34, 4), (2, 16), (1, 2)), uf(1026 + 8, (UW, 4), (10, 16), (1, 2)), allu, [("A", "cvt")])
            cp(cvt(32, (34, 4), (1, 2)), uf(1024, (UW, 4), (1, 2)), allu, [("A", "cvt")])
            b = mbank()
            tr([(PS[b](ci * 128, (1, 128), pn=34), cvt(ci * 34, (1, 34)), IDF) for ci in range(4)],
               [("A", "cvt"), "tabs"], [("ps", b)])
            cp(cst(0, (1, 512), pn=34), PS[b](0, (1, 512), pn=34), [("ps", b)], [("A", "cst")])
            spdma(oconv_s.ap()[l], cst(0, (1, 512), pn=32), [("A", "cst")], [], sem="cst")
            spdma(oconv_p.ap()[l], cst(0, (1, 512), p0=32, pn=2), [("A", "cst")], [], sem="cst")
            wout_group(l, 0, 4, mixb, ("A", "mix"))

            chk(6 + 20 * l)
            pa = af(0)
            pb_ = af(5632)
            dT = ab(11264)
            P.fence(fence_nop)
            wpl, kpl = wload(W["pool_w"].ap()[l].rearrange("g c d -> c g d"), 4)
            pvt = af(13568)
            for gi in range(4):
                wdw = 2 << gi
                src = lambda off, n, gi=gi: zp(gi * PW + off, (1, n))
                src3 = lambda off, n, gi=gi: zp(gi * PW + 1039 + off, (23, 16), (1, n))
                cur, cur3 = src, src3
                bufs = [pa, pb_]
                sh = 1
                step = 0
                lo = 0
                while sh < wdw:
                    ob = bufs[step % 2]
                    kin = [KZ(gi), ("A", "pp", 0), ("A", "pp", 1)]
                    lo2 = lo + sh
                    tt(ob(lo2, (1, 1039 - lo2)), cur(lo2, 1039 - lo2), cur(lo2 - sh, 1039 - lo2), ALU.add,
                       kin, [("A", "pp", step % 2)])
                    tt(ob(1039 + lo2, (23, 16), (1, 23 - lo2)), cur3(lo2, 23 - lo2), cur3(lo2 - sh, 23 - lo2), ALU.add,
                       kin, [("A", "pp", step % 2)])
                    cur = lambda off, n, ob=ob: ob(off, (1, n))
                    cur3 = lambda off, n, ob=ob: ob(1039 + off, (23, 16), (1, n))
                    lo = lo2
                    sh *= 2
                    step += 1
                kin = [KZ(gi), ("A", "pp", 0), ("A", "pp", 1), "tabs"]
                last = ("A", "pp", (step - 1) % 2)
                tt(cur(15, 16), cur(15, 16), TB(TC_CORR + gi * 16, (1, 16)), ALU.mult, kin, [last, ("A", "corr")])
                stt(dT(0, (1, 1024)), cur(15, 1024), 1.0 / wdw, src(15, 1024), ALU.mult, ALU.subtract,
                    kin, [("A", "dT")])
                stt(dT(1024, (8, 16), (1, 8)), cur3(15, 8), 1.0 / wdw, src3(15, 8), ALU.mult, ALU.subtract,
                    kin, [("A", "dT")])
                for t in range(NTT):
                    b = nbank()
                    mm([(PS[b](0, (1, TW)), [(wpl(gi), dT(t * TW, (1, TW)))])], [kpl, ("A", "dT")], [("ps", b)])
                    act(mixb(gi * T + t * TW, (1, TW)), PS[b](0, (1, TW)), AF.Identity, [("ps", b), "vecs"],
                        [("A", "mix")], scale=VC(vb + 108 + gi, (1, 1)))
                cp(pvt(gi * 255, (15, 16), (1, 15)), zp(gi * PW + 1039 + 8, (23, 16), (1, 15)), [KZ(gi)], [("A", "pvt")])
                cp(pvt(gi * 255 + 240, (1, 15)), zp(gi * PW + 1024, (1, 15)), [KZ(gi)], [("A", "pvt")])
            pst = af(55312)
            for blk, (c0, nr) in enumerate(((0, 128), (128, 127))):
                b = mbank()
                tr([(PS[b](gi * 128, (1, 128), pn=nr), pvt(gi * 255 + c0, (1, nr)), IDF) for gi in range(4)],
                   [("A", "pvt"), "tabs"], [("ps", b)])
                cp(pst(blk * 512, (1, 512), pn=nr), PS[b](0, (1, 512), pn=nr), [("ps", b)], [("A", "cst")])
            spdma(opool_s.ap()[l][0:128, :], pst(0, (1, 512)), [("A", "cst")], [], sem="cst")
            spdma(opool_s.ap()[l][128:240, :], pst(512, (1, 512), pn=112), [("A", "cst")], [], sem="cst")
            spdma(opool_p.ap()[l], pst(512, (1, 512), p0=112, pn=15), [("A", "cst")], [], sem="cst")
            wout_group(l, 512, 4, mixb, ("A", "mix"))

            chk(7 + 20 * l)
            P.fence(fence_nop)
            kT = ab(0)
            qT = ab(4608)
            kdt = ab(9216)
            vtm = ab(13824)
            sgt = ab(18432)
            mixc = ab(23040)
            rt = af(27648)
            vtt = ab(30720)
            s7 = af(32256)
            Sst = af(40448)
            Sbf = ab(42496)
            scT = ab(43520)
            osb = af(44032)
            junk = af(45056)
            onb = af(46080)
            ytm = ab(47104)
            stat = af(48128)
            qdc = ab(48192)
            qex = ab(48704)
            csb = ab(52800)
            usl = af(56896)
            kds = ab(63040)

            def tm_pair(l, col0, func, dst, dkey, chunks):
                def ev(t, b1, b2):
                    act(vtt(0, (1, TW)), PS[b1](0, (1, TW)), func, [("ps", b1)], [("A", "vtt", 0)])
                    act(vtt(TW, (1, TW)), PS[b2](0, (1, TW)), func, [("ps", b2)], [("A", "vtt", 1)])
                    for jj in range(3):
                        j = t * 3 + jj
                        if j not in chunks:
                            continue
                        hh = h7()
                        tr([(PS7L[hh](d2 * 128, (1, 128)), vtt(d2 * TW + jj * 128, (1, 128)), IDB(0, (1, 128)))
                            for d2 in range(2)], [("A", "vtt", 0), ("A", "vtt", 1), "idb"], [("ps7", hh)])
                        cp(dst(j * 256, (1, 256)), PS7L[hh](0, (1, 256)), [("ps7", hh)], [dkey])
                pair_mm(l, col0, col0 + 128, ev)

            def v_tile_pair(l, h, chunks):
                tm_pair(l, 4096 + h * 256, AF.Identity, vtm, ("A", "vtm"), chunks)

            def k_transposes(h, chunks, phase1):
                for j in chunks:
                    hh = h7()
                    tr([(PS7L[hh](d2 * 128, (1, 128)), kT(d2 * T + j * 128, (1, 128)), IDB(0, (1, 128)))
                        for d2 in range(2)], [("A", "kT"), "idb"], [("ps7", hh)])
                    if phase1:
                        sc = TB(TC_K1 + j * 4 + h, (1, 1))
                    elif j < 8:
                        sc = TB(TC_K2 + h, (1, 1))
                    else:
                        sc = TB(TC_KS + h, (1, 1))
                    act(kdt(j * 256, (1, 256)), PS7L[hh](0, (1, 256)), AF.Identity, [("ps7", hh), "tabs"],
                        [("A", "kdt")], scale=sc)

            for h in range(4):
                rot_pair(l, 3072 + h * 256, kT, ("A", "kT"), rt)
                v_tile_pair(l, h, range(8))
                k_transposes(h, range(8), True)
                b = nbank()
                mm([(PS[b](dc * 256, (1, 256)),
                     [(kdt(j * 256 + dc * 128, (1, 128)), vtm(j * 256, (1, 256))) for j in range(8)])
                    for dc in range(2)], [("A", "kdt"), ("A", "vtm")], [("ps", b)])
                cp(s7(h * 512, (1, 512)), PS[b](0, (1, 512)), [("ps", b)], [("A", "s7")])
            P.dma("pool", lambda e: e.dma_start(out=cc2_in[l].ap(), in_=s7(0, (1, 2048))), ("cc2i", l),
                  [("A", "s7")], [("cc2in", l)])
            if not NOCC:
                P.dma("pool", lambda e: e.collective_compute("AllGather", ALU.bypass, replica_groups=PAIRS,
                                                             ins=[cc2_in[l].ap().opt()], outs=[cc2_out[l].ap().opt()]),
                      ("cc2", l), [("cc2in", l)], [("cc2out", l)], inc=1)
            cc2_src = cc2_in if NOCC else cc2_out
            cc2_key = ("cc2in", l) if NOCC else ("cc2out", l)

            chk(8 + 20 * l)
            P.op("dve", lambda e: e.memset(qex(0, (1, 2048)), 0.0), [("A", "qex")], [("A", "qex")])
            for h in range(4):
                rot_pair(l, 2048 + h * 256, qT, ("A", "qT"), rt)
                rot_pair(l, 3072 + h * 256, kT, ("A", "kT"), rt)
                v_tile_pair(l, h, range(9))
                k_transposes(h, range(9), False)
                tm_pair(l, 5120 + h * 256, AF.Silu, sgt, ("A", "sgt"), range(9))
                spdma(Sst(0, (1, 512)), cc2_src[l].ap()[0:128, h * 512:(h + 1) * 512], [cc2_key], [("A", "S")], sem="Sst")
                ts(Sst(0, (1, 512)), Sst(0, (1, 512)), isb, None, ALU.mult, None, [("A", "S"), "tabs"], [("A", "S")])
                act(Sbf(0, (1, 512)), Sst(0, (1, 512)), AF.Copy, [("A", "S")], [("A", "Sbf")])
                g128 = float(GAM[h] ** 128)
                g8 = float(GAM[h] ** 8)
                P.mark("L%dh%d_chunks" % (l, h))
                cb_ctr = [0]

                def cbank():
                    b = cb_ctr[0] % 6
                    cb_ctr[0] += 1
                    return b

                def scores(j):
                    b = cbank()
                    mm([(PS[b](0, (1, 128)), [(kT(dc * T + j * 128, (1, 128)), qT(dc * T + j * 128, (1, 128)))
                                               for dc in range(2)])], [("A", "kT"), ("A", "qT")], [("ps", b)])
                    return b

                def premask(j, b):
                    s_ = j % 2
                    mk = TB((TC_MS if j == 8 else TC_MP) + h * 128, (1, 128))
                    tt(scT(s_ * 128, (1, 128)), PS[b](0, (1, 128)), mk, ALU.mult, [("ps", b), "tabs"], [("A", "scT", s_)])
                    if j < 8:
                        tt(qdc(0, (128, 2), (1, 128)), qT(j * 128, (T, 2), (1, 128)),
                           TB(TC_QP + h * 128, (0, 2), (1, 128)), ALU.mult, [("A", "qT"), "tabs"], [("A", "qdc")])

                def gn1(j, bo):
                    act(osb(0, (1, 256)), PS[bo](0, (1, 256)), AF.Copy, [("ps", bo)], [("A", "osb")])
                    P.op("act", lambda e: e.activation(junk(0, (1, 256)), osb(0, (1, 256)), AF.Identity,
                                                       scale=-1.0 / 256.0, accum_out=stat(1, (1, 1))),
                         [("A", "osb")], [("A", "junk"), ("A", "stat")])
                    P.op("act", lambda e: e.activation(junk(0, (1, 256)), osb(0, (1, 256)), AF.Square,
                                                       bias=stat(1, (1, 1)), accum_out=stat(2, (1, 1))),
                         [("A", "osb"), ("A", "stat")], [("A", "junk"), ("A", "stat")])
                    act(stat(3, (1, 1)), stat(2, (1, 1)), AF.Sqrt, [("A", "stat")], [("A", "stat3")],
                        bias=float(EPS), scale=1.0 / 256.0)

                def gn2(j):
                    pass

                def gn3(j):
                    ys = j % 2
                    P.op("dve", lambda e: e.reciprocal(stat(4, (1, 1)), stat(3, (1, 1))), [("A", "stat3")], [("A", "stat")])
                    stt(onb(0, (1, 256)), osb(0, (1, 256)), stat(1, (1, 1)), sgt(j * 256, (1, 256)), ALU.add, ALU.mult,
                        [("A", "osb"), ("A", "stat"), ("A", "sgt")], [("A", "onb")])
                    ts(ytm(ys * 256, (1, 256)), onb(0, (1, 256)), stat(4, (1, 1)), None, ALU.mult, None,
                       [("A", "onb"), ("A", "stat")], [("A", "ytm", ys)])
                    hh = h7()
                    tr([(PS7L[hh](d2 * 128, (1, 128)), ytm(ys * 256 + d2 * 128, (1, 128)), IDB(0, (1, 128))) for d2 in range(2)],
                       [("A", "ytm", ys), "idb"], [("ps7", hh)])
                    act(mixc(j * 128, (T, 2), (1, 128)), PS7L[hh](0, (128, 2), (1, 128)), AF.Copy, [("ps7", hh)],
                        [("A", "mixc")])

                def upd_mm(j):
                    bu = cbank()
                    mm([(PS[bu](dc * 256, (1, 256)), [(kdt(j * 256 + dc * 128, (1, 128)), vtm(j * 256, (1, 256)))])
                        for dc in range(2)], [("A", "kdt"), ("A", "vtm")], [("ps", bu)])
                    return bu

                bs = scores(0)
                premask(0, bs)
                bu_next = upd_mm(0)
                for j in range(9):
                    samp = (j == 8)
                    s_ = j % 2
                    bo = cbank()
                    if not samp:
                        mm([(PS[bo](0, (1, 256)),
                             [(scT(s_ * 128, (1, 128)), vtm(j * 256, (1, 256)))] +
                             [(qdc(dc * 128, (1, 128)), Sbf(dc * 256, (1, 256))) for dc in range(2)])],
                           [("A", "scT", s_), ("A", "vtm"), ("A", "qdc"), ("A", "Sbf")], [("ps", bo)])
                        bu = bu_next
                        stt(Sst(0, (1, 512)), Sst(0, (1, 512)), g128, PS[bu](0, (1, 512)), ALU.mult, ALU.add,
                            [("A", "S"), ("ps", bu)], [("A", "S")])
                        if j < 7:
                            act(Sbf(0, (1, 512)), Sst(0, (1, 512)), AF.Copy, [("A", "S")], [("A", "Sbf")])
                            bu_next = upd_mm(j + 1)
                        else:
                            spdma(oret_p.ap()[l, h].rearrange("(c p) e -> p c e", p=128), Sst(0, (256, 2), (1, 256)),
                                  [("A", "S")], [], sem="Sst")
                        bs = scores(j + 1)
                        premask(j + 1, bs)
                        if j >= 1:
                            gn3(j - 1)
                        gn1(j, bo)
                    else:
                        o0_, l0_, r0_ = PS[bo](0, (1, 256)), scT(s_ * 128, (1, 128)), vtm(j * 256, (1, 256))
                        P.op("pe", lambda e, o0_=o0_, l0_=l0_, r0_=r0_: e.matmul(o0_, l0_, r0_, start=True, stop=False),
                             [("A", "scT", s_), ("A", "vtm")], [("ps", bo)])
                        for dc in range(2):
                            tt(qex(0, (136, 16), (1, 8)), qT(dc * T + 1024, (8, 16), (1, 8)),
                               TB(TC_QS + h * 128, (8, 16), (1, 8)), ALU.mult, [("A", "qT"), "tabs"], [("A", "qex")])
                            for g4 in range(4):
                                cs_ = (dc * 4 + g4) % 2
                                d_out = csb(cs_ * 1024, (256, 4), (1, 256))
                                d_in = sret_d.ap()[l, g4 * 4:g4 * 4 + 4, h, dc * 128:(dc + 1) * 128, :].rearrange("s p e -> p s e")
                                P.dma("pool", lambda e, d_out=d_out, d_in=d_in: e.dma_start(out=d_out, in_=d_in),
                                      ("csb", cs_), [], [("A", "csb", cs_)])
                                for s4 in range(4):
                                    s = g4 * 4 + s4
                                    o_, l_, r_ = PS[bo](0, (1, 256)), qex(s * 128, (1, 128)), csb(cs_ * 1024 + s4 * 256, (1, 256))
                                    last_ = (dc == 1 and s == 15)
                                    P.op("pe", lambda e, o_=o_, l_=l_, r_=r_, last_=last_: e.matmul(o_, l_, r_, start=False, stop=last_),
                                         [("A", "qex"), ("A", "csb", cs_)], [("ps", bo)])
                        gn3(j - 1)
                        gn1(j, bo)
                gn3(8)
                P.mark("L%dh%d_supd" % (l, h))
                def uload(u):
                    s, dc = u // 2, u % 2
                    spdma(usl((u % 3) * 256, (1, 256)), sret_d.ap()[l, s, h, dc * 128:(dc + 1) * 128, :],
                          [], [("A", "uin", u % 3)], sem=("uin", u % 3))
                for u in range(3):
                    uload(u)

                def mask_k(s):
                    k_ = s % 2
                    ts(kds(k_ * 256, (1, 256)), kdt(8 * 256, (1, 256)), TB(TC_OH + s, (1, 1)), None, ALU.mult, None,
                       [("A", "kdt"), "tabs"], [("A", "kds", k_)])
                mask_k(0)
                for s in range(16):
                    k_ = s % 2
                    if s + 1 < 16:
                        mask_k(s + 1)
                    bus = []
                    for dc in range(2):
                        bu = cbank()
                        mm([(PS[bu](0, (1, 256)), [(kds(k_ * 256 + dc * 128, (1, 128)), vtm(8 * 256, (1, 256)))])],
                           [("A", "kds", k_), ("A", "vtm")], [("ps", bu)])
                        bus.append(bu)
                    for dc in range(2):
                        u = 2 * s + dc
                        bu = bus[dc]
                        stt(usl(768 + (u % 3) * 256, (1, 256)), usl((u % 3) * 256, (1, 256)), g8, PS[bu](0, (1, 256)),
                            ALU.mult, ALU.add, [("A", "uin", u % 3), ("ps", bu)], [("A", "uout", u % 3)])
                        spdma(oret_s.ap()[l, s, h, dc * 128:(dc + 1) * 128, :], usl(768 + (u % 3) * 256, (1, 256)),
                              [("A", "uout", u % 3)], [], sem=("uout", u % 3))
                        if u + 3 < 32:
                            uload(u + 3)
                P.mark("L%dh%d_wout" % (l, h))
                wout_group(l, 1024 + h * 256, 2, mixc, ("A", "mixc"), tt_outer=False)
                P.mark("L%dh%d_end" % (l, h))

        def ple(l):
            pT = ab(36352)
            pin = af(40960)
            plt = af(43008)
            for j in range(NCH):
                s_ = j % 2
                spdma(pin(s_ * 256, (1, 256)), p_d.ap()[l, j * 128:(j + 1) * 128, :], [], [("A", "pin", s_)],
                      sem=("pin", s_))
                b = mbank()
                tr([(PS[b](i * 128, (1, 128)), pin(s_ * 256 + i * 128, (1, 128)), IDF) for i in range(2)],
                   [("A", "pin", s_), "tabs"], [("ps", b)])
                cp(pT(j * 128, (T, 2), (1, 128)), PS[b](0, (128, 2), (1, 128)), [("ps", b)], [("A", "pT")])
            for dc in range(16):
                wg, kg = wload(wtile("ple_gate", l, 0, 16, dc * 128), 16)
                wp, kp = wload(wtile("ple_proj", l, 0, 2, dc * 128), 2)
                for t in range(NTT):
                    ba, bb = nbank(), nbank()
                    mm([(PS[ba](0, (1, TW)), [(wg(kc), xb_rhs(kc, t * TW, TW)) for kc in range(16)])],
                       [kg, KX(t)], [("ps", ba)])
                    mm([(PS[bb](0, (1, TW)), [(wp(kc), pT(kc * T + t * TW, (1, TW))) for kc in range(2)])],
                       [kp, ("A", "pT")], [("ps", bb)])
                    s_ = t % 2
                    act(plt(s_ * TW, (1, TW)), PS[ba](0, (1, TW)), AF.Sigmoid, [("ps", ba)], [("A", "plt", s_)])
                    tt(plt(s_ * TW, (1, TW)), plt(s_ * TW, (1, TW)), PS[bb](0, (1, TW)), ALU.mult,
                       [("A", "plt", s_), ("ps", bb)], [("A", "plt", s_)])
                    xr = XR(dc * T + t * TW, (1, TW))
                    tt(xr, xr, plt(s_ * TW, (1, TW)), ALU.add, [("A", "plt", s_), KR(t)], [KR(t)])

        try:
            for l in range(NL):
                chk(1 + 20 * l)
                ffn(l, "ffn1")
                chk(2 + 20 * l)
                layernorm(l, 0)
                chk(3 + 20 * l)
                mixing(l)
                chk(10 + 20 * l)
                layernorm(l, 1)
                chk(11 + 20 * l)
                ffn(l, "ffn2")
                chk(12 + 20 * l)
                ple(l)
                chk(13 + 20 * l)
                layernorm(l, 2, final=(l == NL - 1))
        except _Stop:
            pass

        P.mark("out")
        P.fence(fence_nop)
        for j in range(NCH):
            sl = j % 2
            yo = af(sl * 8192)
            ky = ("A", "yo", sl)
            for q in range(4 if not KSUB & 4 else 0):
                b = mbank()
                tr([(PS[b](i * 128, (1, 128)), XR((q * 4 + i) * T + j * 128, (1, 128)), IDF) for i in range(4)],
                   [KR(j // 3), "tabs"], [("ps", b)])
                cp(yo(q * 512, (1, 512)), PS[b](0, (1, 512)), [("ps", b)], [ky], eng="act" if q % 2 else "dve")
            spdma(y_d.ap()[j * 128:(j + 1) * 128, :], yo(0, (1, 2048)), [ky], [], sem=("yo", sl))

        P.mark("end")
        _NC_CACHE["marks"] = P.marks
        P.emit()
    _NC_CACHE["used"] = set(I.keys()) | set(W.keys())
    _NC_CACHE["outs"] = set(O.keys())
    return nc


def _tables(hf):
    tab = np.zeros((128, NTAB), np.float64)
    pos = np.concatenate([hf * 1024 + np.arange(1024), np.tile(16384 + np.arange(8), 16)]).astype(np.float32)
    inv = (np.float32(10000.0) ** (-(np.arange(128, dtype=np.float32)) / np.float32(128))).astype(np.float32)
    ang = (pos[None, :] * inv[:, None]).astype(np.float32)
    tab[:, TC_COS:TC_COS + T] = np.cos(ang.astype(np.float64))
    tab[:, TC_SIN:TC_SIN + T] = np.sin(ang.astype(np.float64))
    m = np.arange(128)[:, None]
    lq = np.arange(128)[None, :]
    for h in range(4):
        g = GAM[h]
        mp = np.where(lq >= m, 0.0625 * g ** np.maximum(lq - m, 0).astype(np.float64), 0.0)
        tab[:, TC_MP + h * 128:TC_MP + (h + 1) * 128] = mp
        same = (m // 8) == (lq // 8)
        dj = (lq % 8) - (m % 8)
        ms = np.where(same & (dj >= 0), 0.0625 * g ** np.maximum(dj, 0).astype(np.float64), 0.0)
        tab[:, TC_MS + h * 128:TC_MS + (h + 1) * 128] = ms
        tab[:, TC_QP + h * 128:TC_QP + (h + 1) * 128] = (g ** (np.arange(128) + 1.0))[None, :]
        tab[:, TC_QS + h * 128:TC_QS + (h + 1) * 128] = (g ** ((np.arange(128) % 8) + 1.0))[None, :]
        for c in range(8):
            tab[:, TC_K1 + c * 4 + h] = 0.0625 * g ** (1023.0 - 128 * c - np.arange(128))
        tab[:, TC_K2 + h] = 0.0625 * g ** (127.0 - np.arange(128))
        tab[:, TC_KS + h] = 0.0625 * g ** (7.0 - (np.arange(128) % 8))
    for s in range(16):
        tab[:, TC_OH + s] = ((np.arange(128) // 8) == s).astype(np.float64)
    tab[:, TC_ISB] = float(hf)
    for gi in range(4):
        w = 2 << gi
        p16 = hf * 1024 + np.arange(16)
        tab[:, TC_CORR + gi * 16:TC_CORR + (gi + 1) * 16] = (w / np.minimum(w, p16 + 1.0))[None, :]
    tab[:, TC_ID:TC_ID + 128] = np.eye(128)
    tab[:, TC_ONES:TC_ONES + 128] = 1.0 / 2048.0
    return tab.astype(np.float32)


def kernel(x_prompt, x_sample, p_prompt, p_sample, cache_conv, cache_pool, state_ret,
           ln1_g, ln1_b, ffn1_w_gate, ffn1_w_up, ffn1_w_down, w_in, conv_w, pool_w, pool_scale,
           w_out, ln2_g, ln2_b, ffn2_w_gate, ffn2_w_up, ffn2_w_down, ple_gate, ple_proj, ln3_g, ln3_b):
    f = lambda a: np.ascontiguousarray(np.asarray(a, dtype=np.float32))
    x_prompt, x_sample, p_prompt, p_sample = f(x_prompt), f(x_sample), f(p_prompt), f(p_sample)
    cache_conv, cache_pool, state_ret = f(cache_conv), f(cache_pool), f(state_ret)
    vecs = np.zeros((128, NV), np.float32)
    for l in range(NL):
        base = l * 112
        for k, a in enumerate((ln1_g, ln1_b, ln2_g, ln2_b, ln3_g, ln3_b)):
            vecs[:, base + k * 16:base + (k + 1) * 16] = f(a)[l].reshape(16, 128).T
        cw = f(conv_w)[l]
        for ci in range(4):
            for j in range(3):
                vecs[:, base + 96 + ci * 3 + j] = cw[j, ci * 128:(ci + 1) * 128]
        vecs[:, base + 108:base + 112] = f(pool_scale)[l].reshape(4, 128).T
    wts = {"ffn1_w_gate": f(ffn1_w_gate), "ffn1_w_up": f(ffn1_w_up), "ffn1_w_down": f(ffn1_w_down),
           "w_in": f(w_in), "pool_w": f(pool_w), "w_out": f(w_out),
           "ffn2_w_gate": f(ffn2_w_gate), "ffn2_w_up": f(ffn2_w_up), "ffn2_w_down": f(ffn2_w_down),
           "ple_gate": f(ple_gate), "ple_proj": f(ple_proj)}
    tabs = [_tables(0), _tables(1)]
    in_maps = []
    for c in range(8):
        pr, hf = c // 2, c % 2
        sl = slice(16 * c, 16 * c + 16)
        m = {
            "x": np.concatenate([x_prompt[pr, hf * 1024:(hf + 1) * 1024], x_sample[sl].reshape(128, D)], 0),
            "p": np.concatenate([p_prompt[:, pr, hf * 1024:(hf + 1) * 1024], p_sample[:, sl].reshape(NL, 128, 256)], 1),
            "cconv": cache_conv[:, sl].reshape(NL, 32, 512),
            "cpool": cache_pool[:, sl].reshape(NL, 240, 512),
            "sret": state_ret[:, sl],
            "vecs": vecs, "tabs": tabs[hf],
        }
        m = {k: np.ascontiguousarray(v) for k, v in m.items()}
        m.update(wts)
        in_maps.append(m)
    if os.environ.get("KDBG_MAPS"):
        return in_maps
    if "nc" not in _NC_CACHE:
        _NC_CACHE["nc"] = build_program()
    used = _NC_CACHE["used"]
    in_maps = [{k: v for k, v in m.items() if k in used} for m in in_maps]
    res = run_bass_kernel_spmd(_NC_CACHE["nc"], in_maps, core_ids=list(range(8)))
    R = res.results
    y_prompt = np.zeros((4, 2048, D), np.float32)
    y_sample = np.zeros((128, 8, D), np.float32)
    conv_p = np.zeros((NL, 4, 2, 512), np.float32)
    pool_p = np.zeros((NL, 4, 15, 512), np.float32)
    ret_p = np.zeros((NL, 4, 4, 256, 256), np.float32)
    conv_s = np.zeros((NL, 128, 2, 512), np.float32)
    pool_s = np.zeros((NL, 128, 15, 512), np.float32)
    ret_s = np.zeros((NL, 128, 4, 256, 256), np.float32)
    for c in range(8):
        pr, hf = c // 2, c % 2
        sl = slice(16 * c, 16 * c + 16)
        r = R[c]
        y = np.asarray(r["y"])
        y_prompt[pr, hf * 1024:(hf + 1) * 1024] = y[:1024]
        y_sample[sl] = y[1024:].reshape(16, 8, D)
        if "oconv_s" in r:
            conv_s[:, sl] = np.asarray(r["oconv_s"]).reshape(NL, 16, 2, 512)
        if "opool_s" in r:
            pool_s[:, sl] = np.asarray(r["opool_s"]).reshape(NL, 16, 15, 512)
        if "oret_s" in r:
            ret_s[:, sl] = np.asarray(r["oret_s"])
        if hf == 1:
            if "oconv_p" in r:
                conv_p[:, pr] = np.asarray(r["oconv_p"])
            if "opool_p" in r:
                pool_p[:, pr] = np.asarray(r["opool_p"])
            if "oret_p" in r:
                ret_p[:, pr] = np.asarray(r["oret_p"])
    return (y_prompt, y_sample, conv_p, pool_p, ret_p, conv_s, pool_s, ret_s)
```
